# Optimizing a Trainium2 kernel written in Bass

```python
import math
import jax, jax.numpy as jnp
from jax import lax
import numpy as np

D_MODEL = 2048
BATCH = 4
SEQ = 4096
DEPTH = 2

MEM_LEN = 256
HEAD_DIM = 128
A_HEADS = 8
A_PATTERNS = ((128, 1), (512, 4), (2048, 16))
A_BLOCK = 128
N_BUCKETS = 32
MAX_DISTANCE = 2048
B_HEADS = 4
B_DK = 512
B_DV = 1024
B_GATE_RANK = 16
B_GATE_TAU = 16.0
B_CHUNK = 64
C_HEADS = 8
C_Q_RANK = 512
C_KV_RANK = 512
C_NOPE = 128
C_ROPE = 64
C_V = 128
ROPE_THETA = 10000.0
C_BLOCK = 128
X_HEADS = 4
X_HEAD_DIM = D_MODEL // X_HEADS
D_FF = -(-8 * D_MODEL // (3 * 256)) * 256
N_BRANCH = 3
IN_SIZES = (A_HEADS * HEAD_DIM, A_HEADS * HEAD_DIM, A_HEADS * HEAD_DIM,
            B_DK, B_DK, B_DV, B_GATE_RANK, B_DV,
            C_Q_RANK, C_KV_RANK, C_ROPE,
            N_BRANCH * D_MODEL)
D_IN = sum(IN_SIZES)
F32 = jnp.float32
NEG = -1e30
EPS = 1e-6

kernel_name = 'hybrid_gated_dilated_gla_mla_block'


def rmsnorm(x, g):
    xf = x.astype(F32)
    y = xf * lax.rsqrt(jnp.mean(xf * xf, axis=-1, keepdims=True) + EPS)
    return (y * g.astype(F32)).astype(x.dtype)


def split_columns(z):
    idx, acc = [], 0
    for size in IN_SIZES[:-1]:
        acc += size
        idx.append(acc)
    return jnp.split(z, idx, axis=-1)


def t5_bucket(dist):
    max_exact = N_BUCKETS // 2
    n = jnp.maximum(dist, 1).astype(F32)
    large = max_exact + (jnp.log(n / max_exact) / math.log(MAX_DISTANCE / max_exact)
                         * (N_BUCKETS - max_exact)).astype(jnp.int32)
    large = jnp.minimum(large, N_BUCKETS - 1)
    return jnp.where(dist < max_exact, dist, large)


def dilated_attention(q, k, v, rel_bias):
    b, s, h, d = q.shape
    scale = d ** -0.5
    qi = jnp.arange(A_BLOCK)[:, None]
    kj = jnp.arange(2 * A_BLOCK)[None, :]
    steps_back = qi + A_BLOCK - kj
    outs, lses = [], []
    for window, dil in A_PATTERNS:
        n_steps = window // dil
        L = s // dil
        nb = -(-L // A_BLOCK)
        Lp = nb * A_BLOCK

        def to_blocks(t):
            t = t.reshape(b, L, dil, h, d).transpose(0, 2, 1, 3, 4)
            t = jnp.pad(t, ((0, 0), (0, 0), (0, Lp - L), (0, 0), (0, 0)))
            return t.reshape(b, dil, nb, A_BLOCK, h, d)

        def with_prev(t):
            prev = jnp.pad(t[:, :, :-1], ((0, 0), (0, 0), (1, 0), (0, 0), (0, 0), (0, 0)))
            return jnp.concatenate([prev, t], axis=3)

        qb = to_blocks(q)
        kb = with_prev(to_blocks(k))
        vb = with_prev(to_blocks(v))
        key_idx = jnp.arange(nb)[:, None, None] * A_BLOCK + kj[None] - A_BLOCK
        valid = (steps_back >= 0) & (steps_back <= n_steps) & (key_idx >= 0)
        dist = jnp.clip(steps_back, 0, n_steps) * dil
        bias = rel_bias[t5_bucket(dist)].astype(F32).transpose(2, 0, 1)
        logits = jnp.einsum('brnqhd,brnkhd->brnhqk', qb, kb).astype(F32) * scale + bias
        logits = jnp.where(valid[None, None, :, None], logits, NEG)
        lse = jax.nn.logsumexp(logits, axis=-1)
        p = jnp.exp(logits - lse[..., None]).astype(v.dtype)
        o = jnp.einsum('brnhqk,brnkhd->brnqhd', p, vb)
        o = o.reshape(b, dil, Lp, h, d)[:, :, :L].transpose(0, 2, 1, 3, 4).reshape(b, s, h, d)
        lse = lse.transpose(0, 1, 2, 4, 3).reshape(b, dil, Lp, h)[:, :, :L]
        lse = lse.transpose(0, 2, 1, 3).reshape(b, s, h)
        outs.append(o)
        lses.append(lse)
    wts = jax.nn.softmax(jnp.stack(lses, axis=0), axis=0)
    out = jnp.sum(wts[..., None] * jnp.stack(outs, axis=0).astype(F32), axis=0)
    return out.astype(q.dtype)


def gated_linear_attention(q, k, v, log_a, r, norm_g):
    b, s, h, dk = q.shape
    dv = v.shape[-1]
    c = B_CHUNK
    n = s // c
    qc = (q.astype(F32) * dk ** -0.5).reshape(b, n, c, h, dk)
    kc = k.astype(F32).reshape(b, n, c, h, dk)
    vc = v.astype(F32).reshape(b, n, c, h, dv)
    cum = jnp.cumsum(log_a.astype(F32).reshape(b, n, c, h, dk), axis=2)
    cum_last = cum[:, :, -1:]
    q_dec = qc * jnp.exp(cum)
    k_inv = kc * jnp.exp(-cum)
    k_state = kc * jnp.exp(cum_last - cum)
    causal = jnp.tril(jnp.ones((c, c), dtype=bool))
    att = jnp.where(causal, jnp.einsum('bnihk,bnjhk->bnhij', q_dec, k_inv), 0.0)
    o_intra = jnp.einsum('bnhij,bnjhv->bnihv', att, vc)
    d_state = jnp.einsum('bnjhk,bnjhv->nbhkv', k_state, vc)
    decay = jnp.exp(cum_last[:, :, 0]).transpose(1, 0, 2, 3)

    def step(state, inp):
        dec, ds = inp
        return dec[..., None] * state + ds, state

    _, prev_states = lax.scan(step, jnp.zeros((b, h, dk, dv), F32), (decay, d_state))
    o_inter = jnp.einsum('bnihk,nbhkv->bnihv', q_dec, prev_states)
    o = (o_intra + o_inter).reshape(b, s, h, dv)
    o = rmsnorm(o, norm_g) * jax.nn.silu(r.astype(F32))
    return o.reshape(b, s, h * dv).astype(v.dtype)


def rope_tables(s):
    pos = jnp.arange(s, dtype=F32)
    inv = ROPE_THETA ** (-jnp.arange(0, C_ROPE, 2, dtype=F32) / C_ROPE)
    ang = pos[:, None] * inv[None, :]
    return jnp.cos(ang), jnp.sin(ang)


def apply_rope(x, cos, sin):
    half = x.shape[-1] // 2
    x1 = x[..., :half].astype(F32)
    x2 = x[..., half:].astype(F32)
    return jnp.concatenate([x1 * cos - x2 * sin, x1 * sin + x2 * cos], axis=-1).astype(x.dtype)


def latent_attention(c_qa, c_kva, c_kr, q_a_norm, w_qb, kv_a_norm, w_kvb):
    b, s, _ = c_qa.shape
    q = (rmsnorm(c_qa, q_a_norm) @ w_qb).reshape(b, s, C_HEADS, C_NOPE + C_ROPE)
    kv = (rmsnorm(c_kva, kv_a_norm) @ w_kvb).reshape(b, s, C_HEADS, C_NOPE + C_V)
    cos, sin = rope_tables(s)
    q = jnp.concatenate([q[..., :C_NOPE], apply_rope(q[..., C_NOPE:], cos[:, None], sin[:, None])], axis=-1)
    k_rope = apply_rope(c_kr, cos, sin)
    k = jnp.concatenate([kv[..., :C_NOPE],
                         jnp.broadcast_to(k_rope[:, :, None], (b, s, C_HEADS, C_ROPE))], axis=-1)
    v = kv[..., C_NOPE:]
    scale = (C_NOPE + C_ROPE) ** -0.5
    nb = s // C_BLOCK
    q_blocks = q.reshape(b, nb, C_BLOCK, C_HEADS, C_NOPE + C_ROPE).transpose(1, 0, 2, 3, 4)
    k_pos = jnp.arange(s)

    def one_block(args):
        qblk, start = args
        logits = jnp.einsum('bqhd,bkhd->bhqk', qblk, k).astype(F32) * scale
        q_pos = start + jnp.arange(C_BLOCK)
        logits = jnp.where(k_pos[None, :] <= q_pos[:, None], logits, NEG)
        p = jax.nn.softmax(logits, axis=-1).astype(v.dtype)
        return jnp.einsum('bhqk,bkhd->bqhd', p, v)

    o = lax.map(one_block, (q_blocks, jnp.arange(nb) * C_BLOCK))
    return o.transpose(1, 0, 2, 3, 4).reshape(b, s, C_HEADS * C_V)


def memory_cross_attention(hx, mem_n, w_xq, w_xkv, w_xo):
    b, s, _ = hx.shape
    m = mem_n.shape[1]
    q = (hx @ w_xq).reshape(b, s, X_HEADS, X_HEAD_DIM)
    kv = (mem_n @ w_xkv).reshape(b, m, 2, X_HEADS, X_HEAD_DIM)
    logits = jnp.einsum('bshd,bmhd->bhsm', q, kv[:, :, 0]).astype(F32) * X_HEAD_DIM ** -0.5
    p = jax.nn.softmax(logits, axis=-1).astype(hx.dtype)
    o = jnp.einsum('bhsm,bmhd->bshd', p, kv[:, :, 1]).reshape(b, s, X_HEADS * X_HEAD_DIM)
    return o @ w_xo


def setup_inputs(seed: int = 0) -> dict:
    key = jax.random.key(seed)
    ks = iter(jax.random.split(key, 40))
    L, D = DEPTH, D_MODEL

    def w(shape, fan_in):
        return jax.random.normal(next(ks), shape, F32) * fan_in ** -0.5

    def gain(shape):
        return 1.0 + 0.02 * jax.random.normal(next(ks), shape, F32)

    def small(shape, sc):
        return sc * jax.random.normal(next(ks), shape, F32)

    return {
        'x': jax.random.normal(next(ks), (BATCH, SEQ, D), F32),
        'mem': jax.random.normal(next(ks), (BATCH, MEM_LEN, D), F32),
        'rel_bias': small((N_BUCKETS, A_HEADS), 0.5),
        'norm_mix': gain((L, D)),
        'w_in': w((L, D, D_IN), D),
        'b_gate': small((L, N_BRANCH * D), 0.02),
        'w_alpha': w((L, B_GATE_RANK, B_DK), B_GATE_RANK),
        'b_alpha': small((L, B_DK), 0.1),
        'gla_norm': gain((L, B_HEADS, B_DV // B_HEADS)),
        'q_a_norm': gain((L, C_Q_RANK)),
        'w_qb': w((L, C_Q_RANK, C_HEADS * (C_NOPE + C_ROPE)), C_Q_RANK),
        'kv_a_norm': gain((L, C_KV_RANK)),
        'w_kvb': w((L, C_KV_RANK, C_HEADS * (C_NOPE + C_V)), C_KV_RANK),
        'w_up_a': w((L, A_HEADS * HEAD_DIM, D), A_HEADS * HEAD_DIM),
        'w_up_b': w((L, B_DV, D), B_DV),
        'w_up_c': w((L, C_HEADS * C_V, D), C_HEADS * C_V),
        'w_o': w((L, D, D), D),
        'norm_x': gain((L, D)),
        'norm_mem': gain((L, D)),
        'w_xq': w((L, D, X_HEADS * X_HEAD_DIM), D),
        'w_xkv': w((L, D, 2 * X_HEADS * X_HEAD_DIM), D),
        'w_xo': w((L, X_HEADS * X_HEAD_DIM, D), X_HEADS * X_HEAD_DIM),
        'norm_ffn': gain((L, D)),
        'w_ffn_gate': w((L, D, D_FF), D),
        'w_ffn_up': w((L, D, D_FF), D),
        'w_ffn_down': w((L, D_FF, D), D_FF),
        'norm_final': gain((D,)),
    }


def reference(x, mem, rel_bias, norm_mix, w_in, b_gate, w_alpha, b_alpha, gla_norm,
              q_a_norm, w_qb, kv_a_norm, w_kvb, w_up_a, w_up_b, w_up_c, w_o,
              norm_x, norm_mem, w_xq, w_xkv, w_xo,
              norm_ffn, w_ffn_gate, w_ffn_up, w_ffn_down, norm_final):
    b, s, d = x.shape
    for l in range(DEPTH):
        h = rmsnorm(x, norm_mix[l])
        z = h @ w_in[l]
        (aq, ak, av, bq, bk, bv, b_lr, br, c_qa, c_kva, c_kr, gates) = split_columns(z)
        o_a = dilated_attention(aq.reshape(b, s, A_HEADS, HEAD_DIM), ak.reshape(b, s, A_HEADS, HEAD_DIM),
                                av.reshape(b, s, A_HEADS, HEAD_DIM), rel_bias).reshape(b, s, A_HEADS * HEAD_DIM)
        log_a = jax.nn.log_sigmoid((b_lr @ w_alpha[l] + b_alpha[l]).astype(F32)) / B_GATE_TAU
        o_b = gated_linear_attention(bq.reshape(b, s, B_HEADS, B_DK // B_HEADS),
                                     bk.reshape(b, s, B_HEADS, B_DK // B_HEADS),
                                     bv.reshape(b, s, B_HEADS, B_DV // B_HEADS),
                                     log_a.reshape(b, s, B_HEADS, B_DK // B_HEADS),
                                     br.reshape(b, s, B_HEADS, B_DV // B_HEADS), gla_norm[l])
        o_c = latent_attention(c_qa, c_kva, c_kr, q_a_norm[l], w_qb[l], kv_a_norm[l], w_kvb[l])
        g = jax.nn.sigmoid((gates + b_gate[l]).astype(F32)).astype(x.dtype).reshape(b, s, N_BRANCH, d)
        merged = (g[:, :, 0] * (o_a @ w_up_a[l]) + g[:, :, 1] * (o_b @ w_up_b[l])
                  + g[:, :, 2] * (o_c @ w_up_c[l]))
        x = x + (merged @ w_o[l]).astype(x.dtype)
        x = x + memory_cross_attention(rmsnorm(x, norm_x[l]), rmsnorm(mem, norm_mem[l]),
                                       w_xq[l], w_xkv[l], w_xo[l]).astype(x.dtype)
        hf = rmsnorm(x, norm_ffn[l])
        x = x + ((jax.nn.silu(hf @ w_ffn_gate[l]) * (hf @ w_ffn_up[l])) @ w_ffn_down[l]).astype(x.dtype)
    return rmsnorm(x, norm_final)
```

```python
import os
import math
from contextlib import ExitStack
import numpy as np
import concourse.bass as bass
import concourse.mybir as mybir
from concourse.bass_utils import run_bass_kernel_spmd

F32 = mybir.dt.float32
BF16 = mybir.dt.bfloat16
AF = mybir.ActivationFunctionType
ALU = mybir.AluOpType

ENGS = ("pe", "act", "dve", "pool", "sp")
SEM_LIMIT = 24000

D = 2048
KC = 16
TO = 2048
TC = 4096
DFF = 5632
MEM = 256
O_AQ, O_AK, O_AV, O_BQ, O_BK, O_BV, O_BLR, O_BR, O_CQA, O_CKVA, O_CKR, O_G = (
    0, 1024, 2048, 3072, 3584, 4096, 5120, 5136, 6160, 6672, 7184, 7248)
DIN = 13392
EPS = 1e-6
NEGB = -30000.0


class SemPool:
    def __init__(self, nc):
        self.nc = nc
        self.stack = ExitStack()
        self.free = []
        self.n = 0

    def get(self):
        if self.free:
            return self.free.pop()
        self.n += 1
        obj = self.stack.enter_context(self.nc.semaphore(f"s{self.n}"))
        return [obj, 0]

    def put(self, ent):
        if ent[1] < SEM_LIMIT:
            self.free.append(ent)

    def close(self):
        self.stack.close()


_POOLS = {}


class Prog:
    def __init__(self, nc, name):
        self.nc = nc
        self.name = name
        self.pool = _POOLS[id(nc)]
        self.stack = ExitStack()
        self.ops = {e: [] for e in ENGS}
        self.esem = {}
        self.ecount = {e: 0 for e in ENGS}
        self.nsem = 0
        self.sem_objs = {}
        self.sem_ent = {}
        for e in ENGS:
            self._new_esem(e)
        self.waited = {e: {} for e in ENGS}
        self.last_write = {}
        self.readers = {}
        self.dsem = {}
        self.dcount = {}
        self.all_dtok = {}
        self.n_inst = 0
        self.rr = 0

    def _sem(self, name):
        ent = self.pool.get()
        self.nsem += 1
        self.sem_objs[self.nsem] = ent[0]
        self.sem_ent[self.nsem] = ent
        return self.nsem

    def _new_esem(self, e):
        self.esem[e] = self._sem("e")
        self.ecount[e] = self.sem_ent[self.esem[e]][1]

    def sb(self, name, shape, dtype):
        return self.stack.enter_context(self.nc.sbuf_tensor(f"{self.name}_{name}", list(shape), dtype))

    def ps(self, name, shape, dtype=F32):
        return self.stack.enter_context(self.nc.psum_tensor(f"{self.name}_{name}", list(shape), dtype))

    def _deps(self, reads, writes):
        toks = []
        for r in reads:
            t = self.last_write.get(r)
            if t is not None:
                toks.append(t)
        for w in writes:
            t = self.last_write.get(w)
            if t is not None:
                toks.append(t)
            toks.extend(self.readers.get(w, {}).items())
        return toks

    def _commit(self, tok, reads, writes):
        for r in reads:
            d = self.readers.setdefault(r, {})
            if d.get(tok[0], -1) < tok[1]:
                d[tok[0]] = tok[1]
        for w in writes:
            self.last_write[w] = tok
            self.readers[w] = {}

    def _waits(self, eng, toks, skip_sem=None):
        best = {}
        for s, v in toks:
            if s == skip_sem:
                continue
            if best.get(s, -1) < v:
                best[s] = v
        out = []
        wd = self.waited[eng]
        for s, v in best.items():
            if wd.get(s, -1) >= v:
                continue
            wd[s] = v
            out.append((s, v))
        return out

    def op(self, eng, fn, reads=(), writes=()):
        if self.ecount[eng] >= SEM_LIMIT:
            self._new_esem(eng)
        toks = self._deps(reads, writes)
        waits = self._waits(eng, toks, skip_sem=self.esem[eng] if eng == "pe" else None)
        self.ecount[eng] += 1
        my = (self.esem[eng], self.ecount[eng])
        self.sem_ent[my[0]][1] = my[1]
        self._commit(my, reads, writes)
        sems = self.sem_objs

        def emit(e, waits=waits, my=my, fn=fn):
            for s, v in waits:
                e.wait_ge(sems[s], v)
            fn(e).then_inc(sems[my[0]], 1)

        self.ops[eng].append(emit)
        self.n_inst += 1
        return my

    def dma(self, q, out, in_, reads=(), writes=(), key=None, **kw):
        if key is None:
            key = writes[0] if writes else reads[0]
        if key not in self.dsem or self.dcount[key] >= SEM_LIMIT:
            self.dsem[key] = self._sem("d")
            self.dcount[key] = self.sem_ent[self.dsem[key]][1]
        toks = self._deps(reads, writes)
        waits = self._waits(q, toks)
        self.dcount[key] += 16
        my = (self.dsem[key], self.dcount[key])
        self.sem_ent[my[0]][1] = my[1]
        self.all_dtok[my[0]] = my[1]
        self._commit(my, reads, writes)
        sems = self.sem_objs

        def emit(e, waits=waits, my=my):
            for s, v in waits:
                e.wait_ge(sems[s], v)
            e.dma_start(out=out, in_=in_, **kw).then_inc(sems[my[0]], 16)

        self.ops[q].append(emit)
        self.n_inst += 1
        return my

    def finish(self):
        waits = self._waits("sp", list(self.all_dtok.items()))
        sems = self.sem_objs

        def emit_w(e, waits=waits):
            for s, v in waits:
                e.wait_ge(sems[s], v)

        self.ops["sp"].append(emit_w)
        ops = self.ops
        with self.nc.Block() as block:
            @block.sync
            def _(e):
                for f in ops["sp"]:
                    f(e)

            @block.tensor
            def _(e):
                for f in ops["pe"]:
                    f(e)

            @block.scalar
            def _(e):
                for f in ops["act"]:
                    f(e)

            @block.vector
            def _(e):
                for f in ops["dve"]:
                    f(e)

            @block.gpsimd
            def _(e):
                for f in ops["pool"]:
                    f(e)
        self.stack.close()
        for ent in self.sem_ent.values():
            self.pool.put(ent)
        return self.n_inst


def mm(P, out, lhsT, rhs, start, stop, reads, writes):
    P.op("pe", lambda e: e.matmul(out, lhsT, rhs, start=start, stop=stop), reads=reads, writes=writes)


class WStream:
    def __init__(self, P, kc, nbuf=2, width=512, tag="w"):
        self.P = P
        self.kc = kc
        self.tag = tag
        self.bufs = [P.sb(f"{tag}{i}", [128, kc, width], BF16) for i in range(nbuf)]
        self.i = 0

    def load(self, W, c0, ncols, r0=0):
        P = self.P
        j = self.i % len(self.bufs)
        self.i += 1
        buf = self.bufs[j]
        key = (self.tag, j)
        for k0 in range(0, self.kc, 8):
            k1 = min(self.kc, k0 + 8)
            stage_cast(P, buf[:, k0:k1, 0:ncols],
                       W[r0 + k0 * 128:r0 + k1 * 128, c0:c0 + ncols].rearrange("(kc p) c -> p kc c", p=128),
                       k1 - k0, ncols, key)
        return buf, key


def stage_cast(P, dst, src, a, b, key):
    if not hasattr(P, "_wst"):
        P._wst = Rot(P, "wst", [128, 8, 512], F32, 1)
    s_t, sk = P._wst.next()
    P.dma("sp", s_t[:, 0:a, 0:b], src, writes=[sk])
    P.op("pool", lambda e: e.tensor_copy(out=dst, in_=s_t[:, 0:a, 0:b]), reads=[sk], writes=[key])


class Rot:
    def __init__(self, P, name, shape, dtype, n, psum=False):
        self.tiles = [(P.ps if psum else P.sb)(f"{name}{i}", shape, dtype) for i in range(n)]
        self.name = name
        self.i = 0

    def next(self):
        j = self.i % len(self.tiles)
        self.i += 1
        return self.tiles[j], (self.name, j)


def rms_scratch(P, kc, tw, src_dt, tag="r"):
    return dict(xs=P.sb(f"{tag}x", [128, kc, tw], src_dt), sq=P.sb(f"{tag}q", [128, kc, tw], BF16),
                rstd=P.sb(f"{tag}r", [128, tw], F32), pss=P.ps(f"{tag}p", [128, tw]))


def rms_fm(P, src, kc, T, gs, dst, dkey, scr, ones, tw=512):
    xs, sq, rstd, pss = scr["xs"], scr["sq"], scr["rstd"], scr["pss"]
    n_feat = kc * 128
    for n in range(T // tw):
        t0 = n * tw
        for k0 in range(0, kc, 8):
            k1 = min(kc, k0 + 8)
            P.dma("sp", xs[:, k0:k1, 0:tw], src[k0 * 128:k1 * 128, t0:t0 + tw].rearrange("(kc p) t -> p kc t", p=128),
                  writes=[("rx", k) for k in range(kc)])
        for k in range(kc):
            P.op("act", lambda e, k=k: e.activation(out=sq[:, k, 0:tw], in_=xs[:, k, 0:tw], func=AF.Square),
                 reads=[("rx", k)], writes=[("rq", k)])
        for k in range(kc):
            mm(P, pss[:, 0:tw], ones[:], sq[:, k, 0:tw], k == 0, k == kc - 1, ["ones", ("rq", k)], ["rp"])
        P.op("act", lambda e: e.activation(out=rstd[:, 0:tw], in_=pss[:, 0:tw], func=AF.Sqrt, scale=1.0 / n_feat, bias=EPS),
             reads=["rp"], writes=["rr"])
        P.op("dve", lambda e: e.reciprocal(out=rstd[:, 0:tw], in_=rstd[:, 0:tw]), reads=["rr"], writes=["rr"])
        for k in range(kc):
            P.op("dve", lambda e, k=k, t0=t0: e.scalar_tensor_tensor(
                out=dst[:, k, t0:t0 + tw], in0=xs[:, k, 0:tw], scalar=gs[:, k:k + 1], in1=rstd[:, 0:tw],
                op0=ALU.mult, op1=ALU.mult),
                reads=[("rx", k), "gs_" + dkey, "rr"], writes=[(dkey, k)])


def load_small(P, name, src, shape, dt=F32, q="sp"):
    t = P.sb(name, shape, dt)
    P.dma(q, t[:], src, writes=[name])
    return t


def make_ones(P, name="ones", dt=BF16):
    ones = P.sb(name, [128, 128], dt)
    P.op("pool", lambda e: e.memset(ones[:], 1.0), writes=[name])
    return ones


def evac_copy(P, i, out, in_, reads, writes):
    if i % 2 == 0:
        P.op("act", lambda e: e.activation(out=out, in_=in_, func=AF.Copy), reads=reads, writes=writes)
    else:
        P.op("dve", lambda e: e.tensor_copy(out=out, in_=in_), reads=reads, writes=writes)


def phase_A(C, half):
    nc = C["nc"]
    P = Prog(nc, f"A{half}")
    T = TO
    src = C["xT_prev"] if half == 0 else C["xT_own"]
    tok0 = 0 if half == 0 else TO
    W = C["w_in"]
    ones = make_ones(P)
    gs = load_small(P, "gs_hT", C["norm_mix"], [128, KC])
    hT = P.sb("hT", [128, KC, T], BF16)
    rms_fm(P, src, KC, T, gs, hT, "hT", rms_scratch(P, KC, 512, F32), ones)
    ws = WStream(P, KC)
    pp = Rot(P, "pp", [128, 512], F32, 4, psum=True)
    st = Rot(P, "st", [128, 512], BF16, 4)
    stf = Rot(P, "stf", [128, 512], F32, 2)
    hkeys = [("hT", k) for k in range(KC)]
    cnt = [0]

    def fm(col0, ncols, dst, kind="copy", bias=None, bias0=0):
        for c0 in range(0, ncols, 512):
            cw = min(512, ncols - c0)
            wb, wkey = ws.load(W, col0 + c0, cw)
            for m0 in range(0, cw, 128):
                mw = min(128, cw - m0)
                for n in range(T // 512):
                    ps, pkey = pp.next()
                    for k in range(KC):
                        mm(P, ps[0:mw, :], wb[:, k, m0:m0 + mw], hT[:, k, n * 512:(n + 1) * 512], k == 0, k == KC - 1,
                           [wkey, ("hT", k)], [pkey])
                    so, skey = st.next()
                    if kind == "sig":
                        bc = bias0 + (c0 + m0) // 128
                        P.op("act", lambda e, so=so, ps=ps, bc=bc: e.activation(
                            out=so[:, :], in_=ps[:, :], func=AF.Sigmoid, bias=bias[:, bc:bc + 1]),
                            reads=[pkey, "bg"], writes=[skey])
                    else:
                        cnt[0] += 1
                        evac_copy(P, cnt[0], so[0:mw, :], ps[0:mw, :], [pkey], [skey])
                    r = c0 + m0
                    P.dma("sp", dst[r:r + mw, tok0 + n * 512:tok0 + (n + 1) * 512] if dst.shape[1] == TC
                          else dst[r:r + mw, n * 512:(n + 1) * 512], so[0:mw, :], reads=[skey], writes=[("dst", id(dst), r, n)],
                          key=skey)

    def tm(col0, ncols, dst_fn):
        for c0 in range(0, ncols, 512):
            wb, wkey = ws.load(W, col0 + c0, 512)
            for t in range(T // 128):
                ps, pkey = pp.next()
                for k in range(KC):
                    mm(P, ps[:, :], hT[:, k, t * 128:(t + 1) * 128], wb[:, k, :], k == 0, k == KC - 1,
                       [wkey, ("hT", k)], [pkey])
                so, skey = st.next()
                cnt[0] += 1
                evac_copy(P, cnt[0], so[:, :], ps[:, :], [pkey], [skey])
                dap = dst_fn(c0, tok0 + t * 128)
                sap = so[:, :].rearrange("p (h d) -> p h d", h=4) if len(dap.shape) == 3 else so[:, :]
                P.dma("sp", dap, sap, reads=[skey], writes=[("dstt", col0, c0, t)], key=skey)

    SK = os.environ.get("K_SKIP", "")
    if "fm" in SK:
        fm = lambda *a, **k: None
    if "tm" in SK:
        tm = lambda *a, **k: None
    wsm = P.sb("wsm", [128, KC, 144], BF16)
    for (a, b, c, n) in (() if "wsm" in SK else ((0, 16, O_BLR, 16), (16, 80, O_CKR, 64), (80, 112, O_CKR + 32, 32), (112, 144, O_CKR, 32))):
        for k0 in range(0, KC, 8):
            stage_cast(P, wsm[:, k0:k0 + 8, a:b], W[k0 * 128:(k0 + 8) * 128, c:c + n].rearrange("(kc p) c -> p kc c", p=128), 8, n, "wsm")
    for n in range(0 if "wsm" in SK else T // 512):
        for (a, b, dst, isf) in ((0, 16, C["BLRT"], True), (16, 80, C["CKRT"], False), (80, 144, C["CKRST"], False)):
            mw = b - a
            ps, pkey = pp.next()
            for k in range(KC):
                mm(P, ps[0:mw, :], wsm[:, k, a:b], hT[:, k, n * 512:(n + 1) * 512], k == 0, k == KC - 1,
                   ["wsm", ("hT", k)], [pkey])
            so, skey = (stf if isf else st).next()
            P.op("dve", lambda e, so=so, ps=ps, mw=mw: e.tensor_copy(out=so[0:mw, :], in_=ps[0:mw, :]), reads=[pkey], writes=[skey])
            P.dma("sp", dst[0:mw, tok0 + n * 512:tok0 + (n + 1) * 512], so[0:mw, :], reads=[skey],
                  writes=[("dsm", a, n)], key=skey)

    fm(O_AK, 1024, C["AKT"])
    fm(O_CKVA, 512, C["CKVAT"])
    AV, BK, BV = C["AV"], C["BK"], C["BV"]
    tm(O_AV, 1024, lambda c0, t0: AV[c0 // 128:c0 // 128 + 4, t0:t0 + 128, :].rearrange("h p d -> p h d"))
    tm(O_BK, 512, lambda c0, t0: BK[t0:t0 + 128, :])
    tm(O_BV, 1024, lambda c0, t0: BV[t0:t0 + 128, c0:c0 + 512])
    if half == 1:
        bg = load_small(P, "bg", C["b_gate"], [128, 48])
        fm(O_AQ, 1024, C["AQT"])
        fm(O_BQ, 512, C["BQT"])
        fm(O_BK, 512, C["BKT"])
        fm(O_BR, 1024, C["BRT"])
        fm(O_CQA, 512, C["CQAT"])
        fm(O_G, 6144, C["GT"], kind="sig", bias=bg)
    return P.finish()


def phase_B(C):
    nc = C["nc"]
    P = Prog(nc, "B")
    ones = make_ones(P)
    prevb = load_small(P, "prevb", C["prevbias"], [128, 1])
    msk = load_small(P, "msk", C["amask"], [128, 2, 128])
    E = P.sb("E", [128, 48, 128], BF16)
    bt = P.sb("bt", [128, 2, 128], F32)
    for j in range(24):
        P.dma("sp", bt[:], C["abias"][:, 2 * j:2 * j + 2, :], writes=["bt"])
        P.op("act", lambda e: e.activation(out=bt[:], in_=bt[:], func=AF.Exp), reads=["bt"], writes=["bt"])
        P.op("dve", lambda e, j=j: e.tensor_tensor(out=E[:, 2 * j:2 * j + 2, :], in0=bt[:], in1=msk[:], op=ALU.mult),
             reads=["bt", "msk"], writes=["E"])
    QT = Rot(P, "QT", [128, TO], BF16, 2)
    KT = Rot(P, "KT", [128, TC], BF16, 2)
    VP = Rot(P, "VP", [128, 32, 128], BF16, 2)
    num = P.sb("num", [128, TO], F32)
    den = P.sb("den", [128, TO], F32)
    oo = Rot(P, "oo", [128, TO], BF16, 2)
    pS = Rot(P, "pS", [128, 2, 128], F32, 2, psum=True)
    pO = Rot(P, "pO", [128, 2, 128], F32, 2, psum=True)
    pT = Rot(P, "pT", [128, 2, 128], F32, 2)
    pB = Rot(P, "pB", [128, 2, 128], BF16, 3)
    scale = 128 ** -0.5
    AV = C["AV"]
    for h in range(8):
        qt, qk = QT.next()
        kt, kk = KT.next()
        P.dma("sp", qt[:], C["AQT"][h * 128:(h + 1) * 128, :], writes=[qk])
        P.dma("sp", kt[:], C["AKT"][h * 128:(h + 1) * 128, :], writes=[kk])
        for g, dil in enumerate((1, 4, 16)):
            vp, vk = VP.next()
            nbc = 32 // dil
            nbo = 16 // dil
            vsrc = AV[h].rearrange("(blk p r) d -> p r blk d", p=128, r=dil)
            for r in range(dil):
                for b0 in range(0, nbc, 8):
                    b1 = min(nbc, b0 + 8)
                    P.dma("sp", vp[:, r * nbc + b0:r * nbc + b1, :], vsrc[:, r, b0:b1, :], writes=[vk])
            for r in range(dil):
                for ob in range(nbo):
                    cb = nbo + ob
                    q0 = ob * 128 * dil + r
                    qsl = slice(q0, q0 + 127 * dil + 1, dil)
                    ps, psk = pS.next()
                    for j, blk in enumerate((cb - 1, cb)):
                        k0 = blk * 128 * dil + r
                        mm(P, ps[:, j, :], kt[:, k0:k0 + 127 * dil + 1:dil], qt[:, qsl], True, True, [kk, qk], [psk])
                    pt, ptk = pT.next()
                    if cb - 1 < nbo:
                        P.op("act", lambda e, pt=pt, ps=ps: e.activation(out=pt[:, 0, :], in_=ps[:, 0, :], func=AF.Exp,
                                                                         scale=scale, bias=prevb[:, 0:1]),
                             reads=[psk, "prevb"], writes=[ptk])
                        P.op("act", lambda e, pt=pt, ps=ps: e.activation(out=pt[:, 1, :], in_=ps[:, 1, :], func=AF.Exp, scale=scale),
                             reads=[psk], writes=[ptk])
                    else:
                        P.op("act", lambda e, pt=pt, ps=ps: e.activation(out=pt[:], in_=ps[:], func=AF.Exp, scale=scale),
                             reads=[psk], writes=[ptk])
                    pb, pbk = pB.next()
                    e0 = (g * 8 + h) * 2
                    P.op("dve", lambda e, pb=pb, pt=pt, e0=e0: e.tensor_tensor(out=pb[:], in0=pt[:], in1=E[:, e0:e0 + 2, :], op=ALU.mult),
                         reads=[ptk, "E"], writes=[pbk])
                    po, pok = pO.next()
                    for j, blk in enumerate((cb - 1, cb)):
                        mm(P, po[:, 0, :], vp[:, r * nbc + blk, :], pb[:, j, :], j == 0, j == 1, [vk, pbk], [pok])
                    for j in range(2):
                        mm(P, po[:, 1, :], ones[:], pb[:, j, :], j == 0, j == 1, ["ones", pbk], [pok])
                    if g == 0:
                        P.op("dve", lambda e, po=po, qsl=qsl: e.tensor_copy(out=num[:, qsl], in_=po[:, 0, :]), reads=[pok], writes=["num"])
                        P.op("dve", lambda e, po=po, qsl=qsl: e.tensor_copy(out=den[:, qsl], in_=po[:, 1, :]), reads=[pok], writes=["den"])
                    else:
                        P.op("dve", lambda e, po=po, qsl=qsl: e.tensor_tensor(out=num[:, qsl], in0=num[:, qsl], in1=po[:, 0, :], op=ALU.add),
                             reads=[pok, "num"], writes=["num"])
                        P.op("dve", lambda e, po=po, qsl=qsl: e.tensor_tensor(out=den[:, qsl], in0=den[:, qsl], in1=po[:, 1, :], op=ALU.add),
                             reads=[pok, "den"], writes=["den"])
        o, ok = oo.next()
        P.op("dve", lambda e: e.reciprocal(out=den[:], in_=den[:]), reads=["den"], writes=["den"])
        P.op("dve", lambda e, o=o: e.tensor_tensor(out=o[:], in0=num[:], in1=den[:], op=ALU.mult), reads=["num", "den"], writes=[ok])
        P.dma("sp", C["OAT"][h * 128:(h + 1) * 128, :], o[:], reads=[ok], writes=[("oat", h)], key=ok)
    return P.finish()


def phase_D(C):
    nc = C["nc"]
    P = Prog(nc, "D")
    ones = make_ones(P)
    prevb = load_small(P, "prevb", C["prevbias"], [128, 1])
    mk = load_small(P, "mk", C["cmask"], [128, 4, 512], BF16)
    gq = load_small(P, "gs_cqn", C["q_a_norm"], [128, 4])
    gkv = load_small(P, "gs_ckvn", C["kv_a_norm"], [128, 4])
    cosT = load_small(P, "cosT", C["cos2T"], [64, TC])
    sinT = load_small(P, "sinT", C["sin2sT"], [64, TC])
    ckvn = P.sb("ckvn", [128, 4, TC], BF16)
    cqn = P.sb("cqn", [128, 4, TO], BF16)
    scr = rms_scratch(P, 4, 512, BF16)
    rms_fm(P, C["CKVAT"], 4, TC, gkv, ckvn, "ckvn", scr, ones)
    rms_fm(P, C["CQAT"], 4, TO, gq, cqn, "cqn", scr, ones)
    kr = P.sb("kr", [64, TC], BF16)
    t1 = P.sb("t1", [64, 512], BF16)
    t2 = P.sb("t2", [64, 512], BF16)
    for n in range(TC // 512):
        sl = slice(n * 512, (n + 1) * 512)
        P.dma("sp", t1[:], C["CKRT"][:, sl], writes=["t1"])
        P.dma("sp", t2[:], C["CKRST"][:, sl], writes=["t2"])
        P.op("dve", lambda e, sl=sl: e.tensor_tensor(out=t1[:], in0=t1[:], in1=cosT[:, sl], op=ALU.mult), reads=["t1", "cosT"], writes=["t1"])
        P.op("dve", lambda e, sl=sl: e.tensor_tensor(out=t2[:], in0=t2[:], in1=sinT[:, sl], op=ALU.mult), reads=["t2", "sinT"], writes=["t2"])
        P.op("dve", lambda e, sl=sl: e.tensor_tensor(out=kr[:, sl], in0=t1[:], in1=t2[:], op=ALU.add), reads=["t1", "t2"], writes=["kr"])
    wq = P.sb("wq", [128, 4, 1536], BF16)
    wqs = P.sb("wqs", [128, 4, 8, 64], BF16)
    wkv = P.sb("wkv", [128, 4, 2048], BF16)
    for k in range(4):
        rows = slice(k * 128, (k + 1) * 128)
        stage_cast(P, wq[:, k, :].rearrange("p (a c) -> p a c", c=512), C["w_qb"][rows, :].rearrange("p (a c) -> p a c", c=512), 3, 512, "wq")
        stage_cast(P, wkv[:, k, :].rearrange("p (a c) -> p a c", c=512), C["w_kvb"][rows, :].rearrange("p (a c) -> p a c", c=512), 4, 512, "wkv")
        wv = C["w_qb"][rows, :].rearrange("p (h c) -> p h c", c=192)
        stage_cast(P, wqs[:, k, :, 0:32], wv[:, :, 160:192], 8, 32, "wqs")
        stage_cast(P, wqs[:, k, :, 32:64], wv[:, :, 128:160], 8, 32, "wqs")
    KTn = Rot(P, "KTn", [128, TC], BF16, 1)
    Vh = Rot(P, "Vh", [128, 32, 128], BF16, 1)
    QTn = Rot(P, "QTn", [128, TO], BF16, 1)
    QTr = Rot(P, "QTr", [64, TO], BF16, 1)
    qa = P.sb("qa", [64, 512], F32)
    qb = P.sb("qb", [64, 512], F32)
    pp = Rot(P, "pp", [128, 512], F32, 1, psum=True)
    pS = Rot(P, "pS", [128, 512], F32, 2, psum=True)
    pO = Rot(P, "pO", [128, 512], F32, 2, psum=True)
    pD = Rot(P, "pD", [128, 512], F32, 2, psum=True)
    pT = Rot(P, "pT", [128, 512], BF16, 3)
    rd = P.sb("rd", [128, 512], F32)
    oo = Rot(P, "oo", [128, 512], BF16, 2)
    scale = 192 ** -0.5
    cnt = 0
    for h in range(8):
        ktn, ktk = KTn.next()
        vh, vhk = Vh.next()
        qtn, qnk = QTn.next()
        qtr, qrk = QTr.next()
        for n in range(TC // 512):
            ps, pk = pp.next()
            for k in range(4):
                mm(P, ps[:], wkv[:, k, h * 256:h * 256 + 128], ckvn[:, k, n * 512:(n + 1) * 512], k == 0, k == 3, ["wkv", ("ckvn", k)], [pk])
            cnt += 1
            evac_copy(P, cnt, ktn[:, n * 512:(n + 1) * 512], ps[:], [pk], [ktk])
        for b4 in range(8):
            ps, pk = pp.next()
            for j in range(4):
                blk = b4 * 4 + j
                for k in range(4):
                    mm(P, ps[:, j * 128:(j + 1) * 128], ckvn[:, k, blk * 128:(blk + 1) * 128], wkv[:, k, h * 256 + 128:h * 256 + 256],
                       k == 0, k == 3, ["wkv", ("ckvn", k)], [pk])
            cnt += 1
            evac_copy(P, cnt, vh[:, b4 * 4:(b4 + 1) * 4, :], ps[:].rearrange("p (j d) -> p j d", j=4), [pk], [vhk])
        for n in range(TO // 512):
            sl = slice(n * 512, (n + 1) * 512)
            csl = slice(TO + n * 512, TO + (n + 1) * 512)
            ps, pk = pp.next()
            for k in range(4):
                mm(P, ps[:], wq[:, k, h * 192:h * 192 + 128], cqn[:, k, sl], k == 0, k == 3, ["wq", ("cqn", k)], [pk])
            cnt += 1
            evac_copy(P, cnt, qtn[:, sl], ps[:], [pk], [qnk])
            ps, pk = pp.next()
            for k in range(4):
                mm(P, ps[0:64, :], wq[:, k, h * 192 + 128:h * 192 + 192], cqn[:, k, sl], k == 0, k == 3, ["wq", ("cqn", k)], [pk])
            P.op("dve", lambda e, ps=ps, csl=csl: e.tensor_tensor(out=qa[:], in0=ps[0:64, :], in1=cosT[:, csl], op=ALU.mult),
                 reads=[pk, "cosT"], writes=["qa"])
            ps, pk = pp.next()
            for k in range(4):
                mm(P, ps[0:64, :], wqs[:, k, h, :], cqn[:, k, sl], k == 0, k == 3, ["wqs", ("cqn", k)], [pk])
            P.op("dve", lambda e, ps=ps, csl=csl: e.tensor_tensor(out=qb[:], in0=ps[0:64, :], in1=sinT[:, csl], op=ALU.mult),
                 reads=[pk, "sinT"], writes=["qb"])
            P.op("dve", lambda e, sl=sl, qtr=qtr: e.tensor_tensor(out=qtr[:, sl], in0=qa[:], in1=qb[:], op=ALU.add),
                 reads=["qa", "qb"], writes=[qrk])
        for qt in range(4):
            sl = slice(qt * 512, (qt + 1) * 512)
            nkb = 16 + 4 * (qt + 1)
            po, pok = pO.next()
            pd, pdk = pD.next()
            for kb in range(nkb):
                ps, psk = pS.next()
                ksl = slice(kb * 128, (kb + 1) * 128)
                mm(P, ps[:], ktn[:, ksl], qtn[:, sl], True, False, [ktk, qnk], [psk])
                mm(P, ps[:], kr[:, ksl], qtr[:, sl], False, True, ["kr", qrk], [psk])
                pt, ptk = pT.next()
                if kb < 16:
                    P.op("act", lambda e, pt=pt, ps=ps: e.activation(out=pt[:], in_=ps[:], func=AF.Exp, scale=scale, bias=prevb[:, 0:1]),
                         reads=[psk, "prevb"], writes=[ptk])
                else:
                    P.op("act", lambda e, pt=pt, ps=ps: e.activation(out=pt[:], in_=ps[:], func=AF.Exp, scale=scale),
                         reads=[psk], writes=[ptk])
                dj = kb - (16 + 4 * qt)
                if dj >= 0:
                    P.op("pool", lambda e, pt=pt, dj=dj: e.tensor_tensor(out=pt[:], in0=pt[:], in1=mk[:, dj, :], op=ALU.mult),
                         reads=[ptk, "mk"], writes=[ptk])
                mm(P, po[:], vh[:, kb, :], pt[:], kb == 0, kb == nkb - 1, [vhk, ptk], [pok])
                mm(P, pd[:], ones[:], pt[:], kb == 0, kb == nkb - 1, ["ones", ptk], [pdk])
            P.op("dve", lambda e, pd=pd: e.reciprocal(out=rd[:], in_=pd[:]), reads=[pdk], writes=["rd"])
            o, ok = oo.next()
            P.op("dve", lambda e, o=o, po=po: e.tensor_tensor(out=o[:], in0=po[:], in1=rd[:], op=ALU.mult), reads=[pok, "rd"], writes=[ok])
            P.dma("sp", C["OCT"][h * 128:(h + 1) * 128, sl], o[:], reads=[ok], writes=[("oct", h, qt)], key=ok)
    return P.finish()


def phase_C(C):
    nc = C["nc"]
    P = Prog(nc, "C")
    onesb = make_ones(P)
    ubd = load_small(P, "ubd", C["ubd"], [128, 128])
    ust = load_small(P, "ust", C["ust"], [128, 128])
    rm = load_small(P, "rm", C["rowmask"], [128, 2])
    gn = load_small(P, "gn", C["gla_norm"], [128, 8])
    wa = P.sb("wa", [17, 512], F32)
    P.dma("sp", wa[0:16, :], C["w_alpha"], writes=["wa"])
    P.dma("sp", wa[16:17, :], C["b_alpha"], writes=["wa"])
    blr = P.sb("blr", [17, TC], F32)
    P.op("pool", lambda e: e.memset(blr[:], 1.0), writes=["blr"])
    P.dma("sp", blr[0:16, :], C["BLRT"], writes=["blr"])
    S = [P.sb(f"S{h}", [128, 256], F32) for h in range(4)]
    Sb = [Rot(P, f"Sb{h}", [128, 256], BF16, 3) for h in range(4)]
    for h in range(4):
        P.op("pool", lambda e, h=h: e.memset(S[h][:], 0.0), writes=[("S", h)])
    kin = Rot(P, "kin", [128, 512], BF16, 2)
    vin = Rot(P, "vin", [128, 1024], BF16, 2)
    qin = Rot(P, "qin", [128, 4, 128], BF16, 2)
    kTin = Rot(P, "kTin", [128, 4, 128], BF16, 2)
    rin = Rot(P, "rin", [128, 8, 128], BF16, 2)
    sp_ = Rot(P, "sp", [128, 512], F32, 2)
    Ff = Rot(P, "Ff", [128, 512], F32, 2)
    ksA = Rot(P, "ksA", [128, 512], BF16, 2)
    ksB = Rot(P, "ksB", [128, 512], BF16, 2)
    E1 = Rot(P, "E1", [128, 128], F32, 3)
    E2 = Rot(P, "E2", [128, 128], F32, 2)
    qd = Rot(P, "qd", [128, 128], BF16, 2)
    ki = Rot(P, "ki", [128, 128], BF16, 2)
    am = Rot(P, "am", [128, 128], BF16, 2)
    sq = Rot(P, "sq", [128, 2, 128], BF16, 2)
    rs = Rot(P, "rs", [128, 128], F32, 2)
    sr = Rot(P, "sr", [128, 2, 128], F32, 2)
    ot = Rot(P, "ot", [128, 2, 128], F32, 2)
    ob = Rot(P, "ob", [128, 2, 128], BF16, 3)
    pX = Rot(P, "pX", [128, 512], F32, 2, psum=True)
    pC = Rot(P, "pC", [128, 128], F32, 2, psum=True)
    pDs = Rot(P, "pDs", [128, 2, 256], F32, 2, psum=True)
    pOo = Rot(P, "pOo", [128, 2, 128], F32, 2, psum=True)
    qscale = 128 ** -0.5
    sbk = [None] * 4
    for h in range(4):
        t, k = Sb[h].next()
        P.op("pool", lambda e, t=t: e.memset(t[:], 0.0), writes=[k])
        sbk[h] = (t, k)
    for tt in range(TC // 128):
        own = tt >= 16
        tsl = slice(tt * 128, (tt + 1) * 128)
        ot0 = (tt - 16) * 128
        k_t, kk = kin.next()
        v_t, vk = vin.next()
        P.dma("sp", k_t[:], C["BK"][tsl, :], writes=[kk])
        P.dma("sp", v_t[:], C["BV"][tsl, :], writes=[vk])
        if own:
            q_t, qk = qin.next()
            kT_t, kTk = kTin.next()
            r_t, rk = rin.next()
            P.dma("sp", q_t[:], C["BQT"][:, ot0:ot0 + 128].rearrange("(h k) t -> k h t", k=128), writes=[qk])
            P.dma("sp", kT_t[:], C["BKT"][:, ot0:ot0 + 128].rearrange("(h k) t -> k h t", k=128), writes=[kTk])
            P.dma("sp", r_t[:], C["BRT"][:, ot0:ot0 + 128].rearrange("(c v) t -> v c t", v=128), writes=[rk])
        px, pxk = pX.next()
        mm(P, px[:], blr[0:17, tsl], wa[0:17, :], True, True, ["blr", "wa"], [pxk])
        s_t, sk = sp_.next()
        P.op("act", lambda e, s_t=s_t, px=px: e.activation(out=s_t[:], in_=px[:], func=AF.Exp, scale=-1.0), reads=[pxk], writes=[sk])
        P.op("act", lambda e, s_t=s_t: e.activation(out=s_t[:], in_=s_t[:], func=AF.Ln, bias=1.0), reads=[sk], writes=[sk])
        pf, pfk = pX.next()
        mm(P, pf[:], ust[:], s_t[:], True, True, ["ust", sk], [pfk])
        f_t, fk = Ff.next()
        P.op("act", lambda e, f_t=f_t, pf=pf: e.activation(out=f_t[:], in_=pf[:], func=AF.Exp, scale=-1.0 / 16), reads=[pfk], writes=[fk])
        a_t, ak = ksA.next()
        b_t, bk = ksB.next()
        P.op("dve", lambda e, a_t=a_t, k_t=k_t, f_t=f_t: e.scalar_tensor_tensor(out=a_t[:], in0=k_t[:], scalar=rm[:, 0:1], in1=f_t[:],
                                                                                 op0=ALU.mult, op1=ALU.mult), reads=[kk, fk, "rm"], writes=[ak])
        P.op("dve", lambda e, b_t=b_t, k_t=k_t, f_t=f_t: e.scalar_tensor_tensor(out=b_t[:], in0=k_t[:], scalar=rm[:, 1:2], in1=f_t[:],
                                                                                 op0=ALU.mult, op1=ALU.mult), reads=[kk, fk, "rm"], writes=[bk])
        for h in range(4):
            hs = slice(h * 128, (h + 1) * 128)
            pc, pck = pC.next()
            mm(P, pc[:], s_t[:, hs], ubd[:], True, True, [sk, "ubd"], [pck])
            e1, e1k = E1.next()
            P.op("act", lambda e, e1=e1, pc=pc: e.activation(out=e1[:], in_=pc[:], func=AF.Exp, scale=-1.0 / 16), reads=[pck], writes=[e1k])
            S0b, S0k = sbk[h]
            pds, pdsk = pDs.next()
            mm(P, pds[:, 0, :], a_t[:, hs], v_t[:, h * 256:(h + 1) * 256], True, True, [ak, vk], [pdsk])
            mm(P, pds[:, 1, :], b_t[:, hs], v_t[:, h * 256:(h + 1) * 256], True, True, [bk, vk], [pdsk])
            P.op("dve", lambda e, h=h, e1=e1, pds=pds: e.scalar_tensor_tensor(out=S[h][:], in0=S[h][:], scalar=e1[:, 63:64], in1=pds[:, 0, :],
                                                                             op0=ALU.mult, op1=ALU.add), reads=[("S", h), e1k, pdsk], writes=[("S", h)])
            S1b, S1k = Sb[h].next()
            P.op("act", lambda e, h=h, S1b=S1b: e.activation(out=S1b[:], in_=S[h][:], func=AF.Copy), reads=[("S", h)], writes=[S1k])
            P.op("dve", lambda e, h=h, e1=e1, pds=pds: e.scalar_tensor_tensor(out=S[h][:], in0=S[h][:], scalar=e1[:, 127:128], in1=pds[:, 1, :],
                                                                             op0=ALU.mult, op1=ALU.add), reads=[("S", h), e1k, pdsk], writes=[("S", h)])
            S2b, S2k = Sb[h].next()
            P.op("act", lambda e, h=h, S2b=S2b: e.activation(out=S2b[:], in_=S[h][:], func=AF.Copy), reads=[("S", h)], writes=[S2k])
            sbk[h] = (S2b, S2k)
            if not own:
                continue
            e2, e2k = E2.next()
            P.op("act", lambda e, e2=e2, pc=pc: e.activation(out=e2[:], in_=pc[:], func=AF.Exp, scale=1.0 / 16), reads=[pck], writes=[e2k])
            qd_t, qdk = qd.next()
            ki_t, kik = ki.next()
            P.op("dve", lambda e, qd_t=qd_t, q_t=q_t, e1=e1, h=h: e.scalar_tensor_tensor(out=qd_t[:], in0=q_t[:, h, :], scalar=qscale, in1=e1[:],
                                                                                         op0=ALU.mult, op1=ALU.mult), reads=[qk, e1k], writes=[qdk])
            P.op("dve", lambda e, ki_t=ki_t, kT_t=kT_t, e2=e2, h=h: e.tensor_tensor(out=ki_t[:], in0=kT_t[:, h, :], in1=e2[:], op=ALU.mult),
                 reads=[kTk, e2k], writes=[kik])
            pa, pak = pC.next()
            mm(P, pa[:], ki_t[:], qd_t[:], True, True, [kik, qdk], [pak])
            am_t, amk = am.next()
            P.op("dve", lambda e, am_t=am_t, pa=pa: e.tensor_tensor(out=am_t[:], in0=pa[:], in1=ubd[:], op=ALU.mult), reads=[pak, "ubd"], writes=[amk])
            po, pok = pOo.next()
            for vc in range(2):
                vs = slice(h * 256 + vc * 128, h * 256 + (vc + 1) * 128)
                mm(P, po[:, vc, :], v_t[:, vs], am_t[:], True, False, [vk, amk], [pok])
                mm(P, po[:, vc, 0:64], S0b[:, vc * 128:(vc + 1) * 128], qd_t[:, 0:64], False, False, [S0k, qdk], [pok])
                mm(P, po[:, vc, 64:128], S1b[:, vc * 128:(vc + 1) * 128], qd_t[:, 64:128], False, True, [S1k, qdk], [pok])
            sq_t, sqk = sq.next()
            P.op("act", lambda e, sq_t=sq_t, po=po: e.activation(out=sq_t[:], in_=po[:], func=AF.Square), reads=[pok], writes=[sqk])
            pq, pqk = pC.next()
            for vc in range(2):
                mm(P, pq[:], onesb[:], sq_t[:, vc, :], vc == 0, vc == 1, ["ones", sqk], [pqk])
            rs_t, rsk = rs.next()
            P.op("act", lambda e, rs_t=rs_t, pq=pq: e.activation(out=rs_t[:], in_=pq[:], func=AF.Sqrt, scale=1.0 / 256, bias=EPS), reads=[pqk], writes=[rsk])
            P.op("dve", lambda e, rs_t=rs_t: e.reciprocal(out=rs_t[:], in_=rs_t[:]), reads=[rsk], writes=[rsk])
            sr_t, srk = sr.next()
            P.op("act", lambda e, sr_t=sr_t, r_t=r_t, h=h: e.activation(out=sr_t[:], in_=r_t[:, 2 * h:2 * h + 2, :], func=AF.Silu), reads=[rk], writes=[srk])
            ot_t, otk = ot.next()
            ob_t, obk = ob.next()
            for vc in range(2):
                P.op("dve", lambda e, ot_t=ot_t, po=po, vc=vc, h=h, rs_t=rs_t: e.scalar_tensor_tensor(
                    out=ot_t[:, vc, :], in0=po[:, vc, :], scalar=gn[:, 2 * h + vc:2 * h + vc + 1], in1=rs_t[:], op0=ALU.mult, op1=ALU.mult),
                    reads=[pok, rsk, "gn"], writes=[otk])
            P.op("pool", lambda e, ob_t=ob_t, ot_t=ot_t, sr_t=sr_t: e.tensor_tensor(out=ob_t[:], in0=ot_t[:], in1=sr_t[:], op=ALU.mult),
                 reads=[otk, srk], writes=[obk])
            P.dma("sp", C["OBT"][h * 256:(h + 1) * 256, ot0:ot0 + 128].rearrange("(c v) t -> v c t", v=128), ob_t[:], reads=[obk],
                  writes=[("obt", h, tt)], key=obk)
    return P.finish()


def proj_residual(P, W, kc, actT, akey, xin, xout, T, ws, pp, tsl0=0, nfb=16, wrows0=0):
    xb = Rot(P, "xb" + akey, [128, T], F32, 2)
    for cb in range(nfb // 4):
        wb, wkey = ws.load(W, cb * 512, 512, r0=wrows0)
        for m in range(4):
            f = cb * 4 + m
            x_t, xk = xb.next()
            P.dma("sp", x_t[:], xin[f * 128:(f + 1) * 128, tsl0:tsl0 + T], writes=[xk])
            for n in range(T // 512):
                ps, pk = pp.next()
                for k in range(kc):
                    mm(P, ps[:], wb[:, k, m * 128:(m + 1) * 128], actT[:, k, n * 512:(n + 1) * 512], k == 0, k == kc - 1,
                       [wkey, (akey, k)], [pk])
                P.op("dve", lambda e, x_t=x_t, ps=ps, n=n: e.tensor_tensor(out=x_t[:, n * 512:(n + 1) * 512], in0=ps[:],
                                                                           in1=x_t[:, n * 512:(n + 1) * 512], op=ALU.add),
                     reads=[pk, xk], writes=[xk])
            P.dma("sp", xout[f * 128:(f + 1) * 128, tsl0:tsl0 + T], x_t[:], reads=[xk], writes=[("xo", f, tsl0)], key=xk)


def phase_E(C):
    nc = C["nc"]
    P = Prog(nc, "E")
    merged = P.sb("merged", [128, KC, TO], BF16)
    oT = P.sb("oT", [128, 8, TO], BF16)
    ws8 = WStream(P, 8, tag="w8")
    pp = Rot(P, "pp", [128, 512], F32, 4, psum=True)
    gt = Rot(P, "gt", [128, TO], BF16, 2)
    tmp = Rot(P, "tmp", [128, 512], BF16, 3)
    for b, (OT, Wup) in enumerate(((C["OAT"], C["w_up_a"]), (C["OBT"], C["w_up_b"]), (C["OCT"], C["w_up_c"]))):
        P.dma("sp", oT[:], OT.rearrange("(kc p) t -> p kc t", p=128), writes=["oT"])
        for cb in range(4):
            wb, wkey = ws8.load(Wup, cb * 512, 512)
            for m in range(4):
                f = cb * 4 + m
                g_t, gk = gt.next()
                P.dma("sp", g_t[:], C["GT"][b * 2048 + f * 128:b * 2048 + (f + 1) * 128, :], writes=[gk])
                for n in range(4):
                    sl = slice(n * 512, (n + 1) * 512)
                    ps, pk = pp.next()
                    for k in range(8):
                        mm(P, ps[:], wb[:, k, m * 128:(m + 1) * 128], oT[:, k, sl], k == 0, k == 7, [wkey, "oT"], [pk])
                    if b == 0:
                        P.op("dve", lambda e, ps=ps, g_t=g_t, sl=sl, f=f: e.tensor_tensor(out=merged[:, f, sl], in0=ps[:], in1=g_t[:, sl], op=ALU.mult),
                             reads=[pk, gk], writes=[("merged", f)])
                    else:
                        t_t, tk = tmp.next()
                        P.op("dve", lambda e, ps=ps, g_t=g_t, sl=sl, t_t=t_t: e.tensor_tensor(out=t_t[:], in0=ps[:], in1=g_t[:, sl], op=ALU.mult),
                             reads=[pk, gk], writes=[tk])
                        P.op("pool", lambda e, t_t=t_t, sl=sl, f=f: e.tensor_tensor(out=merged[:, f, sl], in0=merged[:, f, sl], in1=t_t[:], op=ALU.add),
                             reads=[tk, ("merged", f)], writes=[("merged", f)])
    ws = WStream(P, KC, tag="w16")
    proj_residual(P, C["w_o"], KC, merged, "merged", C["xT_own"], C["X1T"], TO, ws, pp)
    return P.finish()


def phase_F1(C):
    nc = C["nc"]
    P = Prog(nc, "F1")
    ones = make_ones(P)
    gm = load_small(P, "gs_mT", C["norm_mem"], [128, KC])
    gx = load_small(P, "gs_hT", C["norm_x"], [128, KC])
    scr = rms_scratch(P, KC, 512, F32)
    mT = P.sb("mT", [128, KC, MEM], BF16)
    rms_fm(P, C["memT"], KC, MEM, gm, mT, "mT", scr, ones, tw=256)
    ws = WStream(P, KC, tag="w16")
    pp = Rot(P, "pp", [128, 512], F32, 4, psum=True)
    st = Rot(P, "st", [128, 512], BF16, 4)
    cnt = 0
    for cb in range(4):
        wb, wkey = ws.load(C["w_xkv"], cb * 512, 512)
        for m in range(4):
            f = cb * 4 + m
            ps, pk = pp.next()
            for k in range(KC):
                mm(P, ps[:, 0:MEM], wb[:, k, m * 128:(m + 1) * 128], mT[:, k, :], k == 0, k == KC - 1, [wkey, ("mT", k)], [pk])
            so, sk = st.next()
            cnt += 1
            evac_copy(P, cnt, so[:, 0:MEM], ps[:, 0:MEM], [pk], [sk])
            P.dma("sp", C["KXT"][f * 128:(f + 1) * 128, :], so[:, 0:MEM], reads=[sk], writes=[("kxt", f)], key=sk)
    for cb in range(4):
        wb, wkey = ws.load(C["w_xkv"], 2048 + cb * 512, 512)
        for mb in range(2):
            ps, pk = pp.next()
            for k in range(KC):
                mm(P, ps[:], mT[:, k, mb * 128:(mb + 1) * 128], wb[:, k, :], k == 0, k == KC - 1, [wkey, ("mT", k)], [pk])
            so, sk = st.next()
            cnt += 1
            evac_copy(P, cnt, so[:], ps[:], [pk], [sk])
            P.dma("sp", C["VX"][mb * 128:(mb + 1) * 128, cb * 512:(cb + 1) * 512], so[:], reads=[sk], writes=[("vx", mb, cb)], key=sk)
    hT = P.sb("hT", [128, KC, TO], BF16)
    rms_fm(P, C["X1T"], KC, TO, gx, hT, "hT", scr, ones)
    for cb in range(4):
        wb, wkey = ws.load(C["w_xq"], cb * 512, 512)
        for m in range(4):
            f = cb * 4 + m
            for n in range(4):
                ps, pk = pp.next()
                for k in range(KC):
                    mm(P, ps[:], wb[:, k, m * 128:(m + 1) * 128], hT[:, k, n * 512:(n + 1) * 512], k == 0, k == KC - 1, [wkey, ("hT", k)], [pk])
                so, sk = st.next()
                cnt += 1
                evac_copy(P, cnt, so[:], ps[:], [pk], [sk])
                P.dma("sp", C["QXT"][f * 128:(f + 1) * 128, n * 512:(n + 1) * 512], so[:], reads=[sk], writes=[("qxt", f, n)], key=sk)
    return P.finish()


def phase_F2(C):
    nc = C["nc"]
    P = Prog(nc, "F2")
    ones = make_ones(P)
    KxT = P.sb("KxT", [128, 16, MEM], BF16)
    Vx = P.sb("Vx", [128, 2, 2048], BF16)
    P.dma("sp", KxT[:], C["KXT"].rearrange("(f p) m -> p f m", p=128), writes=["KxT"])
    P.dma("sp", Vx[:], C["VX"].rearrange("(mb p) c -> p mb c", p=128), writes=["Vx"])
    ws = WStream(P, KC, tag="w16")
    pp = Rot(P, "pp", [128, 512], F32, 4, psum=True)
    oxT = P.sb("oxT", [128, KC, TO], BF16)
    qh = Rot(P, "qh", [128, 4, TO], BF16, 2)
    pT = Rot(P, "pT", [128, 2, 512], BF16, 2)
    rd = Rot(P, "rd", [128, 512], F32, 2)
    scale = 512 ** -0.5
    for h in range(4):
        q_t, qk = qh.next()
        P.dma("sp", q_t[:], C["QXT"][h * 512:(h + 1) * 512, :].rearrange("(dc p) t -> p dc t", p=128), writes=[qk])
        for n in range(4):
            sl = slice(n * 512, (n + 1) * 512)
            pt, ptk = pT.next()
            for mb in range(2):
                ps, pk = pp.next()
                for dc in range(4):
                    mm(P, ps[:], KxT[:, h * 4 + dc, mb * 128:(mb + 1) * 128], q_t[:, dc, sl], dc == 0, dc == 3, ["KxT", qk], [pk])
                P.op("act", lambda e, pt=pt, ps=ps, mb=mb: e.activation(out=pt[:, mb, :], in_=ps[:], func=AF.Exp, scale=scale), reads=[pk], writes=[ptk])
            ps, pk = pp.next()
            for mb in range(2):
                mm(P, ps[:], ones[:], pt[:, mb, :], mb == 0, mb == 1, ["ones", ptk], [pk])
            r_t, rk = rd.next()
            P.op("dve", lambda e, r_t=r_t, ps=ps: e.reciprocal(out=r_t[:], in_=ps[:]), reads=[pk], writes=[rk])
            for dc in range(4):
                ps, pk = pp.next()
                for mb in range(2):
                    mm(P, ps[:], Vx[:, mb, h * 512 + dc * 128:h * 512 + (dc + 1) * 128], pt[:, mb, :], mb == 0, mb == 1, ["Vx", ptk], [pk])
                P.op("dve", lambda e, ps=ps, r_t=r_t, f=h * 4 + dc, sl=sl: e.tensor_tensor(out=oxT[:, f, sl], in0=ps[:], in1=r_t[:], op=ALU.mult),
                     reads=[pk, rk], writes=[("oxT", h * 4 + dc)])
    proj_residual(P, C["w_xo"], KC, oxT, "oxT", C["X1T"], C["X2T"], TO, ws, pp)
    return P.finish()


def phase_G2(C):
    nc = C["nc"]
    P = Prog(nc, "G2")
    ones = make_ones(P)
    gx = load_small(P, "gs_hT", C["norm_ffn"], [128, KC])
    hT = P.sb("hT", [128, KC, TO], BF16)
    rms_fm(P, C["X2T"], KC, TO, gx, hT, "hT", rms_scratch(P, KC, 256, F32), ones, tw=256)
    wsg = WStream(P, KC, tag="wg")
    wsu = WStream(P, KC, tag="wu")
    pg = Rot(P, "pg", [128, 512], F32, 3, psum=True)
    pu = Rot(P, "pu", [128, 512], F32, 3, psum=True)
    sg = Rot(P, "sg", [128, 512], F32, 3)
    st = Rot(P, "st", [128, 512], BF16, 4)
    for cb in range(DFF // 512):
        wg, wgk = wsg.load(C["w_ffn_gate"], cb * 512, 512)
        wu, wuk = wsu.load(C["w_ffn_up"], cb * 512, 512)
        for m in range(4):
            f = cb * 4 + m
            for n in range(4):
                sl = slice(n * 512, (n + 1) * 512)
                p1, p1k = pg.next()
                p2, p2k = pu.next()
                for k in range(KC):
                    mm(P, p1[:], wg[:, k, m * 128:(m + 1) * 128], hT[:, k, sl], k == 0, k == KC - 1, [wgk, ("hT", k)], [p1k])
                for k in range(KC):
                    mm(P, p2[:], wu[:, k, m * 128:(m + 1) * 128], hT[:, k, sl], k == 0, k == KC - 1, [wuk, ("hT", k)], [p2k])
                s_t, sk = sg.next()
                P.op("act", lambda e, s_t=s_t, p1=p1: e.activation(out=s_t[:], in_=p1[:], func=AF.Silu), reads=[p1k], writes=[sk])
                o_t, ok = st.next()
                P.op("dve", lambda e, o_t=o_t, s_t=s_t, p2=p2: e.tensor_tensor(out=o_t[:], in0=p2[:], in1=s_t[:], op=ALU.mult), reads=[p2k, sk], writes=[ok])
                P.dma("sp", C["ACTT"][f * 128:(f + 1) * 128, sl], o_t[:], reads=[ok], writes=[("actt", f, n)], key=ok)
    return P.finish()


def phase_G3(C):
    nc = C["nc"]
    P = Prog(nc, "G3")
    NK = DFF // 128
    ws = WStream(P, NK, tag="wd")
    pp = Rot(P, "pp", [128, 512], F32, 4, psum=True)
    aT = Rot(P, "aT", [128, NK, 512], BF16, 1)
    xb = Rot(P, "xb", [128, 512], F32, 3)
    for tg in range(TO // 512):
        tsl = slice(tg * 512, (tg + 1) * 512)
        a_t, ak = aT.next()
        for k0 in range(0, NK, 11):
            P.dma("sp", a_t[:, k0:k0 + 11, :], C["ACTT"][k0 * 128:(k0 + 11) * 128, tsl].rearrange("(kc p) t -> p kc t", p=128), writes=[ak])
        for cb in range(4):
            wb, wkey = ws.load(C["w_ffn_down"], cb * 512, 512)
            for m in range(4):
                f = cb * 4 + m
                x_t, xk = xb.next()
                P.dma("sp", x_t[:], C["X2T"][f * 128:(f + 1) * 128, tsl], writes=[xk])
                ps, pk = pp.next()
                for k in range(NK):
                    mm(P, ps[:], wb[:, k, m * 128:(m + 1) * 128], a_t[:, k, :], k == 0, k == NK - 1, [wkey, ak], [pk])
                P.op("dve", lambda e, x_t=x_t, ps=ps: e.tensor_tensor(out=x_t[:], in0=ps[:], in1=x_t[:], op=ALU.add), reads=[pk, xk], writes=[xk])
                P.dma("sp", C["X3T"][f * 128:(f + 1) * 128, tsl], x_t[:], reads=[xk], writes=[("x3", f, tg)], key=xk)
    return P.finish()


def phase_H(C):
    nc = C["nc"]
    P = Prog(nc, "H")
    ones = make_ones(P)
    gs = load_small(P, "gs", C["norm_final"], [128, KC])
    xs = P.sb("xs", [128, KC, 512], F32)
    sq = P.sb("sq", [128, KC, 512], BF16)
    rstd = P.sb("rstd", [128, 512], F32)
    pss = P.ps("pss", [128, 512])
    oo = Rot(P, "oo", [128, 512], F32, 4)
    for n in range(TO // 512):
        sl = slice(n * 512, (n + 1) * 512)
        for k0 in (0, 8):
            P.dma("sp", xs[:, k0:k0 + 8, :], C["X3T"][k0 * 128:(k0 + 8) * 128, sl].rearrange("(kc p) t -> p kc t", p=128),
                  writes=[("xs", k) for k in range(KC)])
        for k in range(KC):
            P.op("act", lambda e, k=k: e.activation(out=sq[:, k, :], in_=xs[:, k, :], func=AF.Square), reads=[("xs", k)], writes=[("sq", k)])
        for k in range(KC):
            mm(P, pss[:], ones[:], sq[:, k, :], k == 0, k == KC - 1, ["ones", ("sq", k)], ["pss"])
        P.op("act", lambda e: e.activation(out=rstd[:], in_=pss[:], func=AF.Sqrt, scale=1.0 / D, bias=EPS), reads=["pss"], writes=["rstd"])
        P.op("dve", lambda e: e.reciprocal(out=rstd[:], in_=rstd[:]), reads=["rstd"], writes=["rstd"])
        for k in range(KC):
            o_t, ok = oo.next()
            P.op("dve", lambda e, k=k, o_t=o_t: e.scalar_tensor_tensor(out=o_t[:], in0=xs[:, k, :], scalar=gs[:, k:k + 1], in1=rstd[:],
                                                                       op0=ALU.mult, op1=ALU.mult), reads=[("xs", k), "gs", "rstd"], writes=[ok])
            P.dma("sp", C["OUTT"][k * 128:(k + 1) * 128, sl], o_t[:], reads=[ok], writes=[("out", k, n)], key=ok)
    return P.finish()


IN_SPECS = [
    ("xT_own", [D, TO], F32), ("xT_prev", [D, TO], F32), ("memT", [D, MEM], F32),
    ("norm_mix", [128, KC], F32), ("w_in", [D, DIN], F32), ("b_gate", [128, 48], F32),
    ("w_alpha", [16, 512], F32), ("b_alpha", [1, 512], F32), ("gla_norm", [128, 8], F32),
    ("q_a_norm", [128, 4], F32), ("w_qb", [512, 1536], F32), ("kv_a_norm", [128, 4], F32), ("w_kvb", [512, 2048], F32),
    ("w_up_a", [1024, D], F32), ("w_up_b", [1024, D], F32), ("w_up_c", [1024, D], F32), ("w_o", [D, D], F32),
    ("norm_x", [128, KC], F32), ("norm_mem", [128, KC], F32), ("w_xq", [D, D], F32), ("w_xkv", [D, 2 * D], F32), ("w_xo", [D, D], F32),
    ("norm_ffn", [128, KC], F32), ("w_ffn_gate", [D, DFF], F32), ("w_ffn_up", [D, DFF], F32), ("w_ffn_down", [DFF, D], F32),
    ("norm_final", [128, KC], F32),
    ("prevbias", [128, 1], F32), ("amask", [128, 2, 128], F32), ("abias", [128, 48, 128], F32),
    ("cmask", [128, 4, 512], BF16), ("cos2T", [64, TC], F32), ("sin2sT", [64, TC], F32),
    ("ubd", [128, 128], F32), ("ust", [128, 128], F32), ("rowmask", [128, 2], F32),
]
SCRATCH = [
    ("AQT", [1024, TO], BF16), ("AKT", [1024, TC], BF16), ("AV", [8, TC, 128], BF16),
    ("BQT", [512, TO], BF16), ("BKT", [512, TO], BF16), ("BK", [TC, 512], BF16), ("BV", [TC, 1024], BF16),
    ("BLRT", [16, TC], F32), ("BRT", [1024, TO], BF16), ("CQAT", [512, TO], BF16), ("CKVAT", [512, TC], BF16),
    ("CKRT", [64, TC], BF16), ("CKRST", [64, TC], BF16), ("GT", [6144, TO], BF16),
    ("OAT", [1024, TO], BF16), ("OBT", [1024, TO], BF16), ("OCT", [1024, TO], BF16),
    ("X1T", [D, TO], F32), ("X2T", [D, TO], F32), ("ACTT", [DFF, TO], BF16),
    ("KXT", [D, MEM], BF16), ("VX", [MEM, D], BF16), ("QXT", [D, TO], BF16),
]
PHASES = ["A0", "A1", "B", "C", "D", "E", "F1", "F2", "G2", "G3", "H"]


def build_layer(debug=(), phases=None):
    nc = bass.Bass("TRN2", target_bir_lowering=False)
    _POOLS[id(nc)] = SemPool(nc)
    C = {"nc": nc}
    for name, shape, dt in IN_SPECS:
        C[name] = nc.dram_tensor(name, shape, dt, kind="ExternalInput").ap()
    for name, shape, dt in SCRATCH:
        kind = "ExternalOutput" if name in debug else "Internal"
        C[name] = nc.dram_tensor(name, shape, dt, kind=kind).ap()
    C["X3T"] = nc.dram_tensor("X3T", [D, TO], F32, kind="ExternalOutput").ap()
    C["OUTT"] = nc.dram_tensor("OUTT", [D, TO], F32, kind="ExternalOutput").ap()
    fns = {"A0": lambda: phase_A(C, 0), "A1": lambda: phase_A(C, 1), "B": lambda: phase_B(C), "C": lambda: phase_C(C),
           "D": lambda: phase_D(C), "E": lambda: phase_E(C), "F1": lambda: phase_F1(C), "F2": lambda: phase_F2(C), "G2": lambda: phase_G2(C),
           "G3": lambda: phase_G3(C), "H": lambda: phase_H(C)}
    tot = 0
    for ph in (phases or PHASES):
        n = fns[ph]()
        tot += n
        if os.environ.get("K_VERBOSE"):
            print("phase", ph, "instr", n, "sems", _POOLS[id(nc)].n, flush=True)
    _POOLS[id(nc)].close()
    return nc


def _t5_bucket(dist):
    dist = np.asarray(dist)
    n = np.maximum(dist, 1).astype(np.float32)
    large = 16 + (np.log(n / np.float32(16)) / np.float32(math.log(2048 / 16)) * np.float32(16)).astype(np.int32)
    large = np.minimum(large, 31)
    return np.where(dist < 16, dist, large)


def _vec(v, n):
    return np.ascontiguousarray(np.asarray(v, np.float32).reshape(n, 128).T)


def _constants():
    k = np.arange(128)[:, None]
    q = np.arange(128)[None, :]
    amask = np.stack([(k >= q), (k <= q)], axis=1).astype(np.float32)
    steps = np.stack([q + 128 - k, q - k], axis=1)
    steps = np.clip(steps, 0, 128)
    qq = np.arange(512)[None, None, :]
    dj = np.arange(4)[None, :, None]
    import ml_dtypes
    cmask = ((k[:, :, None] + 128 * dj) <= qq).astype(np.float32).astype(ml_dtypes.bfloat16)
    same = (k // 64) == (q // 64)
    ubd = (same & (k <= q)).astype(np.float32)
    ust = (same & (k > q)).astype(np.float32)
    p = np.arange(128)
    rowmask = np.stack([(p < 64), (p >= 64)], axis=1).astype(np.float32)
    return amask, steps, cmask, ubd, ust, rowmask


def _rope_tables(half):
    pos = (np.arange(TC) + (half - 1) * TO).astype(np.float32)
    inv = (np.float32(10000.0) ** (-np.arange(0, 64, 2, dtype=np.float32) / np.float32(64))).astype(np.float32)
    ang = pos[None, :] * inv[:, None]
    cos, sin = np.cos(ang).astype(np.float32), np.sin(ang).astype(np.float32)
    cos2 = np.concatenate([cos, cos], 0)
    sin2s = np.concatenate([-sin, sin], 0)
    return np.ascontiguousarray(cos2), np.ascontiguousarray(sin2s)


_NC_CACHE = {}


def _get_nc(debug=(), phases=None):
    key = (tuple(debug), tuple(phases) if phases else None)
    if key not in _NC_CACHE:
        _NC_CACHE[key] = build_layer(debug, phases)
    return _NC_CACHE[key]


def layer_inputs(inp, l, xT_shards, cores):
    amask, steps, cmask, ubd, ust, rowmask = _constants()
    rel = np.asarray(inp["rel_bias"], np.float32)
    ab = np.zeros((128, 48, 128), np.float32)
    for g, dil in enumerate((1, 4, 16)):
        bk = _t5_bucket(steps * dil)
        for h in range(8):
            ab[:, (g * 8 + h) * 2:(g * 8 + h) * 2 + 2, :] = rel[bk, h]
    shared = {
        "norm_mix": _vec(inp["norm_mix"][l], KC), "w_in": np.asarray(inp["w_in"][l]), "b_gate": _vec(inp["b_gate"][l], 48),
        "w_alpha": np.asarray(inp["w_alpha"][l]), "b_alpha": np.asarray(inp["b_alpha"][l]).reshape(1, 512),
        "gla_norm": np.ascontiguousarray(np.asarray(inp["gla_norm"][l], np.float32).reshape(4, 2, 128).transpose(2, 0, 1).reshape(128, 8)),
        "q_a_norm": _vec(inp["q_a_norm"][l], 4), "w_qb": np.asarray(inp["w_qb"][l]),
        "kv_a_norm": _vec(inp["kv_a_norm"][l], 4), "w_kvb": np.asarray(inp["w_kvb"][l]),
        "w_up_a": np.asarray(inp["w_up_a"][l]), "w_up_b": np.asarray(inp["w_up_b"][l]), "w_up_c": np.asarray(inp["w_up_c"][l]),
        "w_o": np.asarray(inp["w_o"][l]), "norm_x": _vec(inp["norm_x"][l], KC), "norm_mem": _vec(inp["norm_mem"][l], KC),
        "w_xq": np.asarray(inp["w_xq"][l]), "w_xkv": np.asarray(inp["w_xkv"][l]), "w_xo": np.asarray(inp["w_xo"][l]),
        "norm_ffn": _vec(inp["norm_ffn"][l], KC), "w_ffn_gate": np.asarray(inp["w_ffn_gate"][l]),
        "w_ffn_up": np.asarray(inp["w_ffn_up"][l]), "w_ffn_down": np.asarray(inp["w_ffn_down"][l]),
        "norm_final": _vec(inp["norm_final"], KC),
        "amask": amask, "abias": ab, "cmask": cmask, "ubd": ubd, "ust": ust, "rowmask": rowmask,
    }
    zeros = np.zeros((D, TO), np.float32)
    maps = []
    for c in cores:
        b, hf = c // 2, c % 2
        cos2, sin2s = _rope_tables(hf)
        m = dict(shared)
        m["xT_own"] = xT_shards[c]
        m["xT_prev"] = xT_shards[c - 1] if hf == 1 else zeros
        m["memT"] = np.ascontiguousarray(np.asarray(inp["mem"][b], np.float32).T)
        m["prevbias"] = np.full((128, 1), 0.0 if hf == 1 else NEGB, np.float32)
        m["cos2T"] = cos2
        m["sin2sT"] = sin2s
        maps.append(m)
    return maps


def kernel(**inp):
    x = np.asarray(inp["x"], np.float32)
    cores = list(range(8))
    xT = [np.ascontiguousarray(x[c // 2, (c % 2) * TO:(c % 2 + 1) * TO, :].T) for c in cores]
    nc = _get_nc()
    res = None
    for l in range(2):
        maps = layer_inputs(inp, l, xT, cores)
        res = run_bass_kernel_spmd(nc, maps, core_ids=cores)
        xT = [np.asarray(r["X3T"]) for r in res.results]
    out = np.empty((4, 4096, D), np.float32)
    for c in cores:
        out[c // 2, (c % 2) * TO:(c % 2 + 1) * TO, :] = np.asarray(res.results[c]["OUTT"]).T
    return out
```

```python
import os
import math
from contextlib import ExitStack
import numpy as np
import concourse.bass as bass
import concourse.mybir as mybir
from concourse.bass_utils import run_bass_kernel_spmd

F32 = mybir.dt.float32
BF16 = mybir.dt.bfloat16
AF = mybir.ActivationFunctionType
ALU = mybir.AluOpType

ENGS = ("pe", "act", "dve", "pool", "sp")
SEM_LIMIT = 24000

D = 2048
KC = 16
TO = 2048
TC = 4096
DFF = 5632
MEM = 256
O_AQ, O_AK, O_AV, O_BQ, O_BK, O_BV, O_BLR, O_BR, O_CQA, O_CKVA, O_CKR, O_G = (
    0, 1024, 2048, 3072, 3584, 4096, 5120, 5136, 6160, 6672, 7184, 7248)
DIN = 13392
EPS = 1e-6
NEGB = -30000.0


class SemPool:
    def __init__(self, nc):
        self.nc = nc
        self.stack = ExitStack()
        self.free = []
        self.n = 0

    def get(self):
        if self.free:
            return self.free.pop()
        self.n += 1
        obj = self.stack.enter_context(self.nc.semaphore(f"s{self.n}"))
        return [obj, 0]

    def put(self, ent):
        if ent[1] < SEM_LIMIT:
            self.free.append(ent)

    def close(self):
        self.stack.close()


_POOLS = {}


class Prog:
    def __init__(self, nc, name):
        self.nc = nc
        Prog._ctr = getattr(Prog, "_ctr", 0) + 1
        self.name = f"{name}u{Prog._ctr}"
        self.pool = _POOLS[id(nc)]
        self.stack = ExitStack()
        self.ops = {e: [] for e in ENGS}
        self.esem = {}
        self.ecount = {e: 0 for e in ENGS}
        self.nsem = 0
        self.sem_objs = {}
        self.sem_ent = {}
        for e in ENGS:
            self._new_esem(e)
        self.waited = {e: {} for e in ENGS}
        self.last_write = {}
        self.readers = {}
        self.dsem = {}
        self.dcount = {}
        self.all_dtok = {}
        self.n_inst = 0
        self.rr = 0

    def _sem(self, name):
        ent = self.pool.get()
        self.nsem += 1
        self.sem_objs[self.nsem] = ent[0]
        self.sem_ent[self.nsem] = ent
        return self.nsem

    def _new_esem(self, e):
        self.esem[e] = self._sem("e")
        self.ecount[e] = self.sem_ent[self.esem[e]][1]

    def sb(self, name, shape, dtype):
        return self.stack.enter_context(self.nc.sbuf_tensor(f"{self.name}_{name}", list(shape), dtype))

    def ps(self, name, shape, dtype=F32):
        return self.stack.enter_context(self.nc.psum_tensor(f"{self.name}_{name}", list(shape), dtype))

    def _deps(self, reads, writes):
        toks = []
        for r in reads:
            t = self.last_write.get(r)
            if t is not None:
                toks.append(t)
        for w in writes:
            t = self.last_write.get(w)
            if t is not None:
                toks.append(t)
            toks.extend(self.readers.get(w, {}).items())
        return toks

    def _commit(self, tok, reads, writes):
        for r in reads:
            d = self.readers.setdefault(r, {})
            if d.get(tok[0], -1) < tok[1]:
                d[tok[0]] = tok[1]
        for w in writes:
            self.last_write[w] = tok
            self.readers[w] = {}

    def _waits(self, eng, toks, skip_sem=None):
        best = {}
        for s, v in toks:
            if s == skip_sem:
                continue
            if best.get(s, -1) < v:
                best[s] = v
        out = []
        wd = self.waited[eng]
        for s, v in best.items():
            if wd.get(s, -1) >= v:
                continue
            wd[s] = v
            out.append((s, v))
        return out

    def op(self, eng, fn, reads=(), writes=()):
        if self.ecount[eng] >= SEM_LIMIT:
            self._new_esem(eng)
        toks = self._deps(reads, writes)
        waits = self._waits(eng, toks, skip_sem=self.esem[eng] if eng == "pe" else None)
        self.ecount[eng] += 1
        my = (self.esem[eng], self.ecount[eng])
        self.sem_ent[my[0]][1] = my[1]
        self._commit(my, reads, writes)
        sems = self.sem_objs

        def emit(e, waits=waits, my=my, fn=fn):
            for s, v in waits:
                e.wait_ge(sems[s], v)
            fn(e).then_inc(sems[my[0]], 1)

        self.ops[eng].append(emit)
        self.n_inst += 1
        return my

    def dma(self, q, out, in_, reads=(), writes=(), key=None, **kw):
        if key is None:
            key = writes[0] if writes else reads[0]
        if key not in self.dsem or self.dcount[key] >= SEM_LIMIT:
            self.dsem[key] = self._sem("d")
            self.dcount[key] = self.sem_ent[self.dsem[key]][1]
        toks = self._deps(reads, writes)
        waits = self._waits(q, toks)
        self.dcount[key] += 16
        my = (self.dsem[key], self.dcount[key])
        self.sem_ent[my[0]][1] = my[1]
        self.all_dtok[my[0]] = my[1]
        self._commit(my, reads, writes)
        sems = self.sem_objs

        def emit(e, waits=waits, my=my):
            for s, v in waits:
                e.wait_ge(sems[s], v)
            e.dma_start(out=out, in_=in_, **kw).then_inc(sems[my[0]], 16)

        self.ops[q].append(emit)
        self.n_inst += 1
        return my

    def custom(self, q, fn, reads=(), writes=(), key=None, inc=1):
        if key not in self.dsem or self.dcount[key] >= SEM_LIMIT:
            self.dsem[key] = self._sem("d")
            self.dcount[key] = self.sem_ent[self.dsem[key]][1]
        toks = self._deps(reads, writes)
        waits = self._waits(q, toks)
        self.dcount[key] += inc
        my = (self.dsem[key], self.dcount[key])
        self.sem_ent[my[0]][1] = my[1]
        self.all_dtok[my[0]] = my[1]
        self._commit(my, reads, writes)
        sems = self.sem_objs

        def emit(e, waits=waits, my=my):
            for s_, v in waits:
                e.wait_ge(sems[s_], v)
            fn(e).then_inc(sems[my[0]], inc)

        self.ops[q].append(emit)
        self.n_inst += 1
        return my

    def finish(self):
        waits = self._waits("sp", list(self.all_dtok.items()))
        sems = self.sem_objs

        def emit_w(e, waits=waits):
            for s, v in waits:
                e.wait_ge(sems[s], v)

        self.ops["sp"].append(emit_w)
        ops = self.ops
        with self.nc.Block() as block:
            @block.sync
            def _(e):
                for f in ops["sp"]:
                    f(e)

            @block.tensor
            def _(e):
                for f in ops["pe"]:
                    f(e)

            @block.scalar
            def _(e):
                for f in ops["act"]:
                    f(e)

            @block.vector
            def _(e):
                for f in ops["dve"]:
                    f(e)

            @block.gpsimd
            def _(e):
                for f in ops["pool"]:
                    f(e)
        self.stack.close()
        for ent in self.sem_ent.values():
            self.pool.put(ent)
        return self.n_inst


def mm(P, out, lhsT, rhs, start, stop, reads, writes):
    P.op("pe", lambda e: e.matmul(out, lhsT, rhs, start=start, stop=stop), reads=reads, writes=writes)


class WStream:
    def __init__(self, P, kc, nbuf=2, width=512, tag="w"):
        self.P = P
        self.kc = kc
        self.tag = tag
        self.bufs = [P.sb(f"{tag}{i}", [128, kc, width], BF16) for i in range(nbuf)]
        self.i = 0

    def load(self, W, c0, ncols, r0=0):
        P = self.P
        j = self.i % len(self.bufs)
        self.i += 1
        buf = self.bufs[j]
        key = (self.tag, j)
        for k0 in range(0, self.kc, 8):
            k1 = min(self.kc, k0 + 8)
            stage_cast(P, buf[:, k0:k1, 0:ncols],
                       W[r0 + k0 * 128:r0 + k1 * 128, c0:c0 + ncols].rearrange("(kc p) c -> p kc c", p=128),
                       k1 - k0, ncols, key)
        return buf, key


def stage_cast(P, dst, src, a, b, key):
    if not hasattr(P, "_wst"):
        P._wst = Rot(P, "wst", [128, 8, 512], F32, 1)
    s_t, sk = P._wst.next()
    P.dma("sp", s_t[:, 0:a, 0:b], src, writes=[sk])
    P.op("pool", lambda e: e.tensor_copy(out=dst, in_=s_t[:, 0:a, 0:b]), reads=[sk], writes=[key])


class Rot:
    def __init__(self, P, name, shape, dtype, n, psum=False):
        self.tiles = [(P.ps if psum else P.sb)(f"{name}{i}", shape, dtype) for i in range(n)]
        self.name = name
        self.i = 0

    def next(self):
        j = self.i % len(self.tiles)
        self.i += 1
        return self.tiles[j], (self.name, j)


def rms_scratch(P, kc, tw, src_dt, tag="r"):
    return dict(xs=P.sb(f"{tag}x", [128, kc, tw], src_dt), sq=P.sb(f"{tag}q", [128, kc, tw], BF16),
                rstd=P.sb(f"{tag}r", [128, tw], F32), pss=P.ps(f"{tag}p", [128, tw]))


def rms_fm(P, src, kc, T, gs, dst, dkey, scr, ones, tw=512):
    xs, sq, rstd, pss = scr["xs"], scr["sq"], scr["rstd"], scr["pss"]
    n_feat = kc * 128
    for n in range(T // tw):
        t0 = n * tw
        for k0 in range(0, kc, 8):
            k1 = min(kc, k0 + 8)
            sap = src(k0, k1, t0, tw) if callable(src) else src[k0 * 128:k1 * 128, t0:t0 + tw].rearrange("(kc p) t -> p kc t", p=128)
            P.dma("sp", xs[:, k0:k1, 0:tw], sap, writes=[("rx", k) for k in range(kc)])
        for k in range(kc):
            P.op("act", lambda e, k=k: e.activation(out=sq[:, k, 0:tw], in_=xs[:, k, 0:tw], func=AF.Square),
                 reads=[("rx", k)], writes=[("rq", k)])
        for k in range(kc):
            mm(P, pss[:, 0:tw], ones[:], sq[:, k, 0:tw], k == 0, k == kc - 1, ["ones", ("rq", k)], ["rp"])
        P.op("act", lambda e: e.activation(out=rstd[:, 0:tw], in_=pss[:, 0:tw], func=AF.Sqrt, scale=1.0 / n_feat, bias=EPS),
             reads=["rp"], writes=["rr"])
        P.op("dve", lambda e: e.reciprocal(out=rstd[:, 0:tw], in_=rstd[:, 0:tw]), reads=["rr"], writes=["rr"])
        for k in range(kc):
            P.op("dve", lambda e, k=k, t0=t0: e.scalar_tensor_tensor(
                out=dst[:, k, t0:t0 + tw], in0=xs[:, k, 0:tw], scalar=gs[:, k:k + 1], in1=rstd[:, 0:tw],
                op0=ALU.mult, op1=ALU.mult),
                reads=[("rx", k), "gs_" + dkey, "rr"], writes=[(dkey, k)])


def load_small(P, name, src, shape, dt=F32, q="sp"):
    t = P.sb(name, shape, dt)
    P.dma(q, t[:], src, writes=[name])
    return t


def make_ones(P, name="ones", dt=BF16):
    ones = P.sb(name, [128, 128], dt)
    P.op("pool", lambda e: e.memset(ones[:], 1.0), writes=[name])
    return ones


def evac_copy(P, i, out, in_, reads, writes):
    if i % 2 == 0:
        P.op("act", lambda e: e.activation(out=out, in_=in_, func=AF.Copy), reads=reads, writes=writes)
    else:
        P.op("dve", lambda e: e.tensor_copy(out=out, in_=in_), reads=reads, writes=writes)


def phase_A(C, half):
    nc = C["nc"]
    P = Prog(nc, f"A{half}")
    T = TO
    src = C["xT_prev"] if half == 0 else C["xT_own"]
    tok0 = 0 if half == 0 else TO
    W = C["w_in"]
    ones = make_ones(P)
    gs = load_small(P, "gs_hT", C["norm_mix"], [128, KC])
    if half == 0:
        hp = load_small(P, "hp", C["hasprev"], [128, 1])
        P.op("dve", lambda e: e.tensor_scalar(out=gs[:], in0=gs[:], scalar1=hp[:, 0:1], scalar2=None, op0=ALU.mult),
             reads=["gs_hT", "hp"], writes=["gs_hT"])
    hT = P.sb("hT", [128, KC, T], BF16)
    rms_fm(P, src, KC, T, gs, hT, "hT", rms_scratch(P, KC, 512, F32), ones)
    ws = WStream(P, KC)
    pp = Rot(P, "pp", [128, 512], F32, 4, psum=True)
    st = Rot(P, "st", [128, 512], BF16, 4)
    stf = Rot(P, "stf", [128, 512], F32, 2)
    hkeys = [("hT", k) for k in range(KC)]
    cnt = [0]

    def fm(col0, ncols, dst, kind="copy", bias=None, bias0=0):
        for c0 in range(0, ncols, 512):
            cw = min(512, ncols - c0)
            wb, wkey = ws.load(W, col0 + c0, cw)
            for m0 in range(0, cw, 128):
                mw = min(128, cw - m0)
                for n in range(T // 512):
                    ps, pkey = pp.next()
                    for k in range(KC):
                        mm(P, ps[0:mw, :], wb[:, k, m0:m0 + mw], hT[:, k, n * 512:(n + 1) * 512], k == 0, k == KC - 1,
                           [wkey, ("hT", k)], [pkey])
                    so, skey = st.next()
                    if kind == "sig":
                        bc = bias0 + (c0 + m0) // 128
                        P.op("act", lambda e, so=so, ps=ps, bc=bc: e.activation(
                            out=so[:, :], in_=ps[:, :], func=AF.Sigmoid, bias=bias[:, bc:bc + 1]),
                            reads=[pkey, "bg"], writes=[skey])
                    else:
                        cnt[0] += 1
                        evac_copy(P, cnt[0], so[0:mw, :], ps[0:mw, :], [pkey], [skey])
                    r = c0 + m0
                    P.dma("sp", dst[r:r + mw, tok0 + n * 512:tok0 + (n + 1) * 512] if dst.shape[1] == TC
                          else dst[r:r + mw, n * 512:(n + 1) * 512], so[0:mw, :], reads=[skey], writes=[("dst", id(dst), r, n)],
                          key=skey)

    def tm(col0, ncols, dst_fn):
        for c0 in range(0, ncols, 512):
            wb, wkey = ws.load(W, col0 + c0, 512)
            for t in range(T // 128):
                ps, pkey = pp.next()
                for k in range(KC):
                    mm(P, ps[:, :], hT[:, k, t * 128:(t + 1) * 128], wb[:, k, :], k == 0, k == KC - 1,
                       [wkey, ("hT", k)], [pkey])
                so, skey = st.next()
                cnt[0] += 1
                evac_copy(P, cnt[0], so[:, :], ps[:, :], [pkey], [skey])
                dap = dst_fn(c0, tok0 + t * 128)
                sap = so[:, :].rearrange("p (h d) -> p h d", h=4) if len(dap.shape) == 3 else so[:, :]
                P.dma("sp", dap, sap, reads=[skey], writes=[("dstt", col0, c0, t)], key=skey)

    SK = os.environ.get("K_SKIP", "")
    if "fm" in SK:
        fm = lambda *a, **k: None
    if "tm" in SK:
        tm = lambda *a, **k: None
    wsm = P.sb("wsm", [128, KC, 144], BF16)
    for (a, b, c, n) in (() if "wsm" in SK else ((0, 16, O_BLR, 16), (16, 80, O_CKR, 64), (80, 112, O_CKR + 32, 32), (112, 144, O_CKR, 32))):
        for k0 in range(0, KC, 8):
            stage_cast(P, wsm[:, k0:k0 + 8, a:b], W[k0 * 128:(k0 + 8) * 128, c:c + n].rearrange("(kc p) c -> p kc c", p=128), 8, n, "wsm")
    for n in range(0 if "wsm" in SK else T // 512):
        for (a, b, dst, isf) in ((0, 16, C["BLRT"], True), (16, 80, C["CKRT"], False), (80, 144, C["CKRST"], False)):
            mw = b - a
            ps, pkey = pp.next()
            for k in range(KC):
                mm(P, ps[0:mw, :], wsm[:, k, a:b], hT[:, k, n * 512:(n + 1) * 512], k == 0, k == KC - 1,
                   ["wsm", ("hT", k)], [pkey])
            so, skey = (stf if isf else st).next()
            P.op("dve", lambda e, so=so, ps=ps, mw=mw: e.tensor_copy(out=so[0:mw, :], in_=ps[0:mw, :]), reads=[pkey], writes=[skey])
            P.dma("sp", dst[0:mw, tok0 + n * 512:tok0 + (n + 1) * 512], so[0:mw, :], reads=[skey],
                  writes=[("dsm", a, n)], key=skey)

    fm(O_AK, 1024, C["AKT"])
    fm(O_CKVA, 512, C["CKVAT"])
    AV, BK, BV = C["AV"], C["BK"], C["BV"]
    tm(O_AV, 1024, lambda c0, t0: AV[c0 // 128:c0 // 128 + 4, t0:t0 + 128, :].rearrange("h p d -> p h d"))
    tm(O_BK, 512, lambda c0, t0: BK[t0:t0 + 128, :])
    tm(O_BV, 1024, lambda c0, t0: BV[t0:t0 + 128, c0:c0 + 512])
    if half == 1:
        bg = load_small(P, "bg", C["b_gate"], [128, 48])
        fm(O_AQ, 1024, C["AQT"])
        fm(O_BQ, 512, C["BQT"])
        fm(O_BK, 512, C["BKT"])
        fm(O_BR, 1024, C["BRT"])
        fm(O_CQA, 512, C["CQAT"])
        fm(O_G, 6144, C["GT"], kind="sig", bias=bg)
    return P.finish()


def phase_B(C):
    nc = C["nc"]
    P = Prog(nc, "B")
    ones = make_ones(P)
    prevb = load_small(P, "prevb", C["prevbias"], [128, 1])
    msk = load_small(P, "msk", C["amask"], [128, 2, 128])
    E = P.sb("E", [128, 48, 128], BF16)
    bt = P.sb("bt", [128, 2, 128], F32)
    for j in range(24):
        P.dma("sp", bt[:], C["abias"][:, 2 * j:2 * j + 2, :], writes=["bt"])
        P.op("act", lambda e: e.activation(out=bt[:], in_=bt[:], func=AF.Exp), reads=["bt"], writes=["bt"])
        P.op("dve", lambda e, j=j: e.tensor_tensor(out=E[:, 2 * j:2 * j + 2, :], in0=bt[:], in1=msk[:], op=ALU.mult),
             reads=["bt", "msk"], writes=["E"])
    QT = Rot(P, "QT", [128, TO], BF16, 2)
    KT = Rot(P, "KT", [128, TC], BF16, 2)
    VP = Rot(P, "VP", [128, 32, 128], BF16, 2)
    num = P.sb("num", [128, TO], F32)
    den = P.sb("den", [128, TO], F32)
    oo = Rot(P, "oo", [128, TO], BF16, 2)
    pS = Rot(P, "pS", [128, 2, 128], F32, 2, psum=True)
    pO = Rot(P, "pO", [128, 2, 128], F32, 2, psum=True)
    pT = Rot(P, "pT", [128, 2, 128], F32, 2)
    pB = Rot(P, "pB", [128, 2, 128], BF16, 3)
    scale = 128 ** -0.5
    AV = C["AV"]
    for h in range(8):
        qt, qk = QT.next()
        kt, kk = KT.next()
        P.dma("sp", qt[:], C["AQT"][h * 128:(h + 1) * 128, :], writes=[qk])
        P.dma("sp", kt[:], C["AKT"][h * 128:(h + 1) * 128, :], writes=[kk])
        for g, dil in enumerate((1, 4, 16)):
            vp, vk = VP.next()
            nbc = 32 // dil
            nbo = 16 // dil
            vsrc = AV[h].rearrange("(blk p r) d -> p r blk d", p=128, r=dil)
            for r in range(dil):
                for b0 in range(0, nbc, 8):
                    b1 = min(nbc, b0 + 8)
                    P.dma("sp", vp[:, r * nbc + b0:r * nbc + b1, :], vsrc[:, r, b0:b1, :], writes=[vk])
            for r in range(dil):
                for ob in range(nbo):
                    cb = nbo + ob
                    q0 = ob * 128 * dil + r
                    qsl = slice(q0, q0 + 127 * dil + 1, dil)
                    ps, psk = pS.next()
                    for j, blk in enumerate((cb - 1, cb)):
                        k0 = blk * 128 * dil + r
                        mm(P, ps[:, j, :], kt[:, k0:k0 + 127 * dil + 1:dil], qt[:, qsl], True, True, [kk, qk], [psk])
                    pt, ptk = pT.next()
                    if cb - 1 < nbo:
                        P.op("act", lambda e, pt=pt, ps=ps: e.activation(out=pt[:, 0, :], in_=ps[:, 0, :], func=AF.Exp,
                                                                         scale=scale, bias=prevb[:, 0:1]),
                             reads=[psk, "prevb"], writes=[ptk])
                        P.op("act", lambda e, pt=pt, ps=ps: e.activation(out=pt[:, 1, :], in_=ps[:, 1, :], func=AF.Exp, scale=scale),
                             reads=[psk], writes=[ptk])
                    else:
                        P.op("act", lambda e, pt=pt, ps=ps: e.activation(out=pt[:], in_=ps[:], func=AF.Exp, scale=scale),
                             reads=[psk], writes=[ptk])
                    pb, pbk = pB.next()
                    e0 = (g * 8 + h) * 2
                    P.op("dve", lambda e, pb=pb, pt=pt, e0=e0: e.tensor_tensor(out=pb[:], in0=pt[:], in1=E[:, e0:e0 + 2, :], op=ALU.mult),
                         reads=[ptk, "E"], writes=[pbk])
                    po, pok = pO.next()
                    for j, blk in enumerate((cb - 1, cb)):
                        mm(P, po[:, 0, :], vp[:, r * nbc + blk, :], pb[:, j, :], j == 0, j == 1, [vk, pbk], [pok])
                    for j in range(2):
                        mm(P, po[:, 1, :], ones[:], pb[:, j, :], j == 0, j == 1, ["ones", pbk], [pok])
                    if g == 0:
                        P.op("dve", lambda e, po=po, qsl=qsl: e.tensor_copy(out=num[:, qsl], in_=po[:, 0, :]), reads=[pok], writes=["num"])
                        P.op("dve", lambda e, po=po, qsl=qsl: e.tensor_copy(out=den[:, qsl], in_=po[:, 1, :]), reads=[pok], writes=["den"])
                    else:
                        P.op("dve", lambda e, po=po, qsl=qsl: e.tensor_tensor(out=num[:, qsl], in0=num[:, qsl], in1=po[:, 0, :], op=ALU.add),
                             reads=[pok, "num"], writes=["num"])
                        P.op("dve", lambda e, po=po, qsl=qsl: e.tensor_tensor(out=den[:, qsl], in0=den[:, qsl], in1=po[:, 1, :], op=ALU.add),
                             reads=[pok, "den"], writes=["den"])
        o, ok = oo.next()
        P.op("dve", lambda e: e.reciprocal(out=den[:], in_=den[:]), reads=["den"], writes=["den"])
        P.op("dve", lambda e, o=o: e.tensor_tensor(out=o[:], in0=num[:], in1=den[:], op=ALU.mult), reads=["num", "den"], writes=[ok])
        P.dma("sp", C["OAT"][h * 128:(h + 1) * 128, :], o[:], reads=[ok], writes=[("oat", h)], key=ok)
    return P.finish()


def phase_D(C):
    nc = C["nc"]
    P = Prog(nc, "D")
    ones = make_ones(P)
    prevb = load_small(P, "prevb", C["prevbias"], [128, 1])
    mk = load_small(P, "mk", C["cmask"], [128, 4, 512], BF16)
    gq = load_small(P, "gs_cqn", C["q_a_norm"], [128, 4])
    gkv = load_small(P, "gs_ckvn", C["kv_a_norm"], [128, 4])
    cosT = load_small(P, "cosT", C["cos2T"], [64, TC])
    sinT = load_small(P, "sinT", C["sin2sT"], [64, TC])
    ckvn = P.sb("ckvn", [128, 4, TC], BF16)
    cqn = P.sb("cqn", [128, 4, TO], BF16)
    scr = rms_scratch(P, 4, 512, BF16)
    rms_fm(P, C["CKVAT"], 4, TC, gkv, ckvn, "ckvn", scr, ones)
    rms_fm(P, C["CQAT"], 4, TO, gq, cqn, "cqn", scr, ones)
    kr = P.sb("kr", [64, TC], BF16)
    t1 = P.sb("t1", [64, 512], BF16)
    t2 = P.sb("t2", [64, 512], BF16)
    for n in range(TC // 512):
        sl = slice(n * 512, (n + 1) * 512)
        P.dma("sp", t1[:], C["CKRT"][:, sl], writes=["t1"])
        P.dma("sp", t2[:], C["CKRST"][:, sl], writes=["t2"])
        P.op("dve", lambda e, sl=sl: e.tensor_tensor(out=t1[:], in0=t1[:], in1=cosT[:, sl], op=ALU.mult), reads=["t1", "cosT"], writes=["t1"])
        P.op("dve", lambda e, sl=sl: e.tensor_tensor(out=t2[:], in0=t2[:], in1=sinT[:, sl], op=ALU.mult), reads=["t2", "sinT"], writes=["t2"])
        P.op("dve", lambda e, sl=sl: e.tensor_tensor(out=kr[:, sl], in0=t1[:], in1=t2[:], op=ALU.add), reads=["t1", "t2"], writes=["kr"])
    wq = P.sb("wq", [128, 4, 1536], BF16)
    wqs = P.sb("wqs", [128, 4, 8, 64], BF16)
    wkv = P.sb("wkv", [128, 4, 2048], BF16)
    for k in range(4):
        rows = slice(k * 128, (k + 1) * 128)
        stage_cast(P, wq[:, k, :].rearrange("p (a c) -> p a c", c=512), C["w_qb"][rows, :].rearrange("p (a c) -> p a c", c=512), 3, 512, "wq")
        stage_cast(P, wkv[:, k, :].rearrange("p (a c) -> p a c", c=512), C["w_kvb"][rows, :].rearrange("p (a c) -> p a c", c=512), 4, 512, "wkv")
        wv = C["w_qb"][rows, :].rearrange("p (h c) -> p h c", c=192)
        stage_cast(P, wqs[:, k, :, 0:32], wv[:, :, 160:192], 8, 32, "wqs")
        stage_cast(P, wqs[:, k, :, 32:64], wv[:, :, 128:160], 8, 32, "wqs")
    KTn = Rot(P, "KTn", [128, TC], BF16, 1)
    Vh = Rot(P, "Vh", [128, 32, 128], BF16, 1)
    QTn = Rot(P, "QTn", [128, TO], BF16, 1)
    QTr = Rot(P, "QTr", [64, TO], BF16, 1)
    qa = P.sb("qa", [64, 512], F32)
    qb = P.sb("qb", [64, 512], F32)
    pp = Rot(P, "pp", [128, 512], F32, 1, psum=True)
    pS = Rot(P, "pS", [128, 512], F32, 2, psum=True)
    pO = Rot(P, "pO", [128, 512], F32, 2, psum=True)
    pD = Rot(P, "pD", [128, 512], F32, 2, psum=True)
    pT = Rot(P, "pT", [128, 512], BF16, 3)
    rd = P.sb("rd", [128, 512], F32)
    oo = Rot(P, "oo", [128, 512], BF16, 2)
    scale = 192 ** -0.5
    cnt = 0
    for h in range(8):
        ktn, ktk = KTn.next()
        vh, vhk = Vh.next()
        qtn, qnk = QTn.next()
        qtr, qrk = QTr.next()
        for n in range(TC // 512):
            ps, pk = pp.next()
            for k in range(4):
                mm(P, ps[:], wkv[:, k, h * 256:h * 256 + 128], ckvn[:, k, n * 512:(n + 1) * 512], k == 0, k == 3, ["wkv", ("ckvn", k)], [pk])
            cnt += 1
            evac_copy(P, cnt, ktn[:, n * 512:(n + 1) * 512], ps[:], [pk], [ktk])
        for b4 in range(8):
            ps, pk = pp.next()
            for j in range(4):
                blk = b4 * 4 + j
                for k in range(4):
                    mm(P, ps[:, j * 128:(j + 1) * 128], ckvn[:, k, blk * 128:(blk + 1) * 128], wkv[:, k, h * 256 + 128:h * 256 + 256],
                       k == 0, k == 3, ["wkv", ("ckvn", k)], [pk])
            cnt += 1
            evac_copy(P, cnt, vh[:, b4 * 4:(b4 + 1) * 4, :], ps[:].rearrange("p (j d) -> p j d", j=4), [pk], [vhk])
        for n in range(TO // 512):
            sl = slice(n * 512, (n + 1) * 512)
            csl = slice(TO + n * 512, TO + (n + 1) * 512)
            ps, pk = pp.next()
            for k in range(4):
                mm(P, ps[:], wq[:, k, h * 192:h * 192 + 128], cqn[:, k, sl], k == 0, k == 3, ["wq", ("cqn", k)], [pk])
            cnt += 1
            evac_copy(P, cnt, qtn[:, sl], ps[:], [pk], [qnk])
            ps, pk = pp.next()
            for k in range(4):
                mm(P, ps[0:64, :], wq[:, k, h * 192 + 128:h * 192 + 192], cqn[:, k, sl], k == 0, k == 3, ["wq", ("cqn", k)], [pk])
            P.op("dve", lambda e, ps=ps, csl=csl: e.tensor_tensor(out=qa[:], in0=ps[0:64, :], in1=cosT[:, csl], op=ALU.mult),
                 reads=[pk, "cosT"], writes=["qa"])
            ps, pk = pp.next()
            for k in range(4):
                mm(P, ps[0:64, :], wqs[:, k, h, :], cqn[:, k, sl], k == 0, k == 3, ["wqs", ("cqn", k)], [pk])
            P.op("dve", lambda e, ps=ps, csl=csl: e.tensor_tensor(out=qb[:], in0=ps[0:64, :], in1=sinT[:, csl], op=ALU.mult),
                 reads=[pk, "sinT"], writes=["qb"])
            P.op("dve", lambda e, sl=sl, qtr=qtr: e.tensor_tensor(out=qtr[:, sl], in0=qa[:], in1=qb[:], op=ALU.add),
                 reads=["qa", "qb"], writes=[qrk])
        for qt in range(4):
            sl = slice(qt * 512, (qt + 1) * 512)
            nkb = 16 + 4 * (qt + 1)
            po, pok = pO.next()
            pd, pdk = pD.next()
            for kb in range(nkb):
                ps, psk = pS.next()
                ksl = slice(kb * 128, (kb + 1) * 128)
                mm(P, ps[:], ktn[:, ksl], qtn[:, sl], True, False, [ktk, qnk], [psk])
                mm(P, ps[:], kr[:, ksl], qtr[:, sl], False, True, ["kr", qrk], [psk])
                pt, ptk = pT.next()
                if kb < 16:
                    P.op("act", lambda e, pt=pt, ps=ps: e.activation(out=pt[:], in_=ps[:], func=AF.Exp, scale=scale, bias=prevb[:, 0:1]),
                         reads=[psk, "prevb"], writes=[ptk])
                else:
                    P.op("act", lambda e, pt=pt, ps=ps: e.activation(out=pt[:], in_=ps[:], func=AF.Exp, scale=scale),
                         reads=[psk], writes=[ptk])
                dj = kb - (16 + 4 * qt)
                if dj >= 0:
                    P.op("pool", lambda e, pt=pt, dj=dj: e.tensor_tensor(out=pt[:], in0=pt[:], in1=mk[:, dj, :], op=ALU.mult),
                         reads=[ptk, "mk"], writes=[ptk])
                mm(P, po[:], vh[:, kb, :], pt[:], kb == 0, kb == nkb - 1, [vhk, ptk], [pok])
                mm(P, pd[:], ones[:], pt[:], kb == 0, kb == nkb - 1, ["ones", ptk], [pdk])
            P.op("dve", lambda e, pd=pd: e.reciprocal(out=rd[:], in_=pd[:]), reads=[pdk], writes=["rd"])
            o, ok = oo.next()
            P.op("dve", lambda e, o=o, po=po: e.tensor_tensor(out=o[:], in0=po[:], in1=rd[:], op=ALU.mult), reads=[pok, "rd"], writes=[ok])
            P.dma("sp", C["OCT"][h * 128:(h + 1) * 128, sl], o[:], reads=[ok], writes=[("oct", h, qt)], key=ok)
    return P.finish()


def phase_C(C):
    nc = C["nc"]
    P = Prog(nc, "C")
    onesb = make_ones(P)
    ubd = load_small(P, "ubd", C["ubd"], [128, 128])
    ust = load_small(P, "ust", C["ust"], [128, 128])
    rm = load_small(P, "rm", C["rowmask"], [128, 2])
    gn = load_small(P, "gn", C["gla_norm"], [128, 8])
    wa = P.sb("wa", [17, 512], F32)
    P.dma("sp", wa[0:16, :], C["w_alpha"], writes=["wa"])
    P.dma("sp", wa[16:17, :], C["b_alpha"], writes=["wa"])
    blr = P.sb("blr", [17, TC], F32)
    P.op("pool", lambda e: e.memset(blr[:], 1.0), writes=["blr"])
    P.dma("sp", blr[0:16, :], C["BLRT"], writes=["blr"])
    S = [P.sb(f"S{h}", [128, 256], F32) for h in range(4)]
    Sb = [Rot(P, f"Sb{h}", [128, 256], BF16, 3) for h in range(4)]
    for h in range(4):
        P.op("pool", lambda e, h=h: e.memset(S[h][:], 0.0), writes=[("S", h)])
    kin = Rot(P, "kin", [128, 512], BF16, 2)
    vin = Rot(P, "vin", [128, 1024], BF16, 2)
    qin = Rot(P, "qin", [128, 4, 128], BF16, 2)
    kTin = Rot(P, "kTin", [128, 4, 128], BF16, 2)
    rin = Rot(P, "rin", [128, 8, 128], BF16, 2)
    sp_ = Rot(P, "sp", [128, 512], F32, 2)
    Ff = Rot(P, "Ff", [128, 512], F32, 2)
    ksA = Rot(P, "ksA", [128, 512], BF16, 2)
    ksB = Rot(P, "ksB", [128, 512], BF16, 2)
    E1 = Rot(P, "E1", [128, 128], F32, 3)
    E2 = Rot(P, "E2", [128, 128], F32, 2)
    qd = Rot(P, "qd", [128, 128], BF16, 2)
    ki = Rot(P, "ki", [128, 128], BF16, 2)
    am = Rot(P, "am", [128, 128], BF16, 2)
    sq = Rot(P, "sq", [128, 2, 128], BF16, 2)
    rs = Rot(P, "rs", [128, 128], F32, 2)
    sr = Rot(P, "sr", [128, 2, 128], F32, 2)
    ot = Rot(P, "ot", [128, 2, 128], F32, 2)
    ob = Rot(P, "ob", [128, 2, 128], BF16, 3)
    pX = Rot(P, "pX", [128, 512], F32, 2, psum=True)
    pC = Rot(P, "pC", [128, 128], F32, 2, psum=True)
    pDs = Rot(P, "pDs", [128, 2, 256], F32, 2, psum=True)
    pOo = Rot(P, "pOo", [128, 2, 128], F32, 2, psum=True)
    qscale = 128 ** -0.5
    sbk = [None] * 4
    for h in range(4):
        t, k = Sb[h].next()
        P.op("pool", lambda e, t=t: e.memset(t[:], 0.0), writes=[k])
        sbk[h] = (t, k)
    for tt in range(TC // 128):
        own = tt >= 16
        tsl = slice(tt * 128, (tt + 1) * 128)
        ot0 = (tt - 16) * 128
        k_t, kk = kin.next()
        v_t, vk = vin.next()
        P.dma("sp", k_t[:], C["BK"][tsl, :], writes=[kk])
        P.dma("sp", v_t[:], C["BV"][tsl, :], writes=[vk])
        if own:
            q_t, qk = qin.next()
            kT_t, kTk = kTin.next()
            r_t, rk = rin.next()
            P.dma("sp", q_t[:], C["BQT"][:, ot0:ot0 + 128].rearrange("(h k) t -> k h t", k=128), writes=[qk])
            P.dma("sp", kT_t[:], C["BKT"][:, ot0:ot0 + 128].rearrange("(h k) t -> k h t", k=128), writes=[kTk])
            P.dma("sp", r_t[:], C["BRT"][:, ot0:ot0 + 128].rearrange("(c v) t -> v c t", v=128), writes=[rk])
        px, pxk = pX.next()
        mm(P, px[:], blr[0:17, tsl], wa[0:17, :], True, True, ["blr", "wa"], [pxk])
        s_t, sk = sp_.next()
        P.op("act", lambda e, s_t=s_t, px=px: e.activation(out=s_t[:], in_=px[:], func=AF.Exp, scale=-1.0), reads=[pxk], writes=[sk])
        P.op("act", lambda e, s_t=s_t: e.activation(out=s_t[:], in_=s_t[:], func=AF.Ln, bias=1.0), reads=[sk], writes=[sk])
        pf, pfk = pX.next()
        mm(P, pf[:], ust[:], s_t[:], True, True, ["ust", sk], [pfk])
        f_t, fk = Ff.next()
        P.op("act", lambda e, f_t=f_t, pf=pf: e.activation(out=f_t[:], in_=pf[:], func=AF.Exp, scale=-1.0 / 16), reads=[pfk], writes=[fk])
        a_t, ak = ksA.next()
        b_t, bk = ksB.next()
        P.op("dve", lambda e, a_t=a_t, k_t=k_t, f_t=f_t: e.scalar_tensor_tensor(out=a_t[:], in0=k_t[:], scalar=rm[:, 0:1], in1=f_t[:],
                                                                                 op0=ALU.mult, op1=ALU.mult), reads=[kk, fk, "rm"], writes=[ak])
        P.op("dve", lambda e, b_t=b_t, k_t=k_t, f_t=f_t: e.scalar_tensor_tensor(out=b_t[:], in0=k_t[:], scalar=rm[:, 1:2], in1=f_t[:],
                                                                                 op0=ALU.mult, op1=ALU.mult), reads=[kk, fk, "rm"], writes=[bk])
        for h in range(4):
            hs = slice(h * 128, (h + 1) * 128)
            pc, pck = pC.next()
            mm(P, pc[:], s_t[:, hs], ubd[:], True, True, [sk, "ubd"], [pck])
            e1, e1k = E1.next()
            P.op("act", lambda e, e1=e1, pc=pc: e.activation(out=e1[:], in_=pc[:], func=AF.Exp, scale=-1.0 / 16), reads=[pck], writes=[e1k])
            S0b, S0k = sbk[h]
            pds, pdsk = pDs.next()
            mm(P, pds[:, 0, :], a_t[:, hs], v_t[:, h * 256:(h + 1) * 256], True, True, [ak, vk], [pdsk])
            mm(P, pds[:, 1, :], b_t[:, hs], v_t[:, h * 256:(h + 1) * 256], True, True, [bk, vk], [pdsk])
            P.op("dve", lambda e, h=h, e1=e1, pds=pds: e.scalar_tensor_tensor(out=S[h][:], in0=S[h][:], scalar=e1[:, 63:64], in1=pds[:, 0, :],
                                                                             op0=ALU.mult, op1=ALU.add), reads=[("S", h), e1k, pdsk], writes=[("S", h)])
            S1b, S1k = Sb[h].next()
            P.op("act", lambda e, h=h, S1b=S1b: e.activation(out=S1b[:], in_=S[h][:], func=AF.Copy), reads=[("S", h)], writes=[S1k])
            P.op("dve", lambda e, h=h, e1=e1, pds=pds: e.scalar_tensor_tensor(out=S[h][:], in0=S[h][:], scalar=e1[:, 127:128], in1=pds[:, 1, :],
                                                                             op0=ALU.mult, op1=ALU.add), reads=[("S", h), e1k, pdsk], writes=[("S", h)])
            S2b, S2k = Sb[h].next()
            P.op("act", lambda e, h=h, S2b=S2b: e.activation(out=S2b[:], in_=S[h][:], func=AF.Copy), reads=[("S", h)], writes=[S2k])
            sbk[h] = (S2b, S2k)
            if not own:
                continue
            e2, e2k = E2.next()
            P.op("act", lambda e, e2=e2, pc=pc: e.activation(out=e2[:], in_=pc[:], func=AF.Exp, scale=1.0 / 16), reads=[pck], writes=[e2k])
            qd_t, qdk = qd.next()
            ki_t, kik = ki.next()
            P.op("dve", lambda e, qd_t=qd_t, q_t=q_t, e1=e1, h=h: e.scalar_tensor_tensor(out=qd_t[:], in0=q_t[:, h, :], scalar=qscale, in1=e1[:],
                                                                                         op0=ALU.mult, op1=ALU.mult), reads=[qk, e1k], writes=[qdk])
            P.op("dve", lambda e, ki_t=ki_t, kT_t=kT_t, e2=e2, h=h: e.tensor_tensor(out=ki_t[:], in0=kT_t[:, h, :], in1=e2[:], op=ALU.mult),
                 reads=[kTk, e2k], writes=[kik])
            pa, pak = pC.next()
            mm(P, pa[:], ki_t[:], qd_t[:], True, True, [kik, qdk], [pak])
            am_t, amk = am.next()
            P.op("dve", lambda e, am_t=am_t, pa=pa: e.tensor_tensor(out=am_t[:], in0=pa[:], in1=ubd[:], op=ALU.mult), reads=[pak, "ubd"], writes=[amk])
            po, pok = pOo.next()
            for vc in range(2):
                vs = slice(h * 256 + vc * 128, h * 256 + (vc + 1) * 128)
                mm(P, po[:, vc, :], v_t[:, vs], am_t[:], True, False, [vk, amk], [pok])
                mm(P, po[:, vc, 0:64], S0b[:, vc * 128:(vc + 1) * 128], qd_t[:, 0:64], False, False, [S0k, qdk], [pok])
                mm(P, po[:, vc, 64:128], S1b[:, vc * 128:(vc + 1) * 128], qd_t[:, 64:128], False, True, [S1k, qdk], [pok])
            sq_t, sqk = sq.next()
            P.op("act", lambda e, sq_t=sq_t, po=po: e.activation(out=sq_t[:], in_=po[:], func=AF.Square), reads=[pok], writes=[sqk])
            pq, pqk = pC.next()
            for vc in range(2):
                mm(P, pq[:], onesb[:], sq_t[:, vc, :], vc == 0, vc == 1, ["ones", sqk], [pqk])
            rs_t, rsk = rs.next()
            P.op("act", lambda e, rs_t=rs_t, pq=pq: e.activation(out=rs_t[:], in_=pq[:], func=AF.Sqrt, scale=1.0 / 256, bias=EPS), reads=[pqk], writes=[rsk])
            P.op("dve", lambda e, rs_t=rs_t: e.reciprocal(out=rs_t[:], in_=rs_t[:]), reads=[rsk], writes=[rsk])
            sr_t, srk = sr.next()
            P.op("act", lambda e, sr_t=sr_t, r_t=r_t, h=h: e.activation(out=sr_t[:], in_=r_t[:, 2 * h:2 * h + 2, :], func=AF.Silu), reads=[rk], writes=[srk])
            ot_t, otk = ot.next()
            ob_t, obk = ob.next()
            for vc in range(2):
                P.op("dve", lambda e, ot_t=ot_t, po=po, vc=vc, h=h, rs_t=rs_t: e.scalar_tensor_tensor(
                    out=ot_t[:, vc, :], in0=po[:, vc, :], scalar=gn[:, 2 * h + vc:2 * h + vc + 1], in1=rs_t[:], op0=ALU.mult, op1=ALU.mult),
                    reads=[pok, rsk, "gn"], writes=[otk])
            P.op("pool", lambda e, ob_t=ob_t, ot_t=ot_t, sr_t=sr_t: e.tensor_tensor(out=ob_t[:], in0=ot_t[:], in1=sr_t[:], op=ALU.mult),
                 reads=[otk, srk], writes=[obk])
            P.dma("sp", C["OBT"][h * 256:(h + 1) * 256, ot0:ot0 + 128].rearrange("(c v) t -> v c t", v=128), ob_t[:], reads=[obk],
                  writes=[("obt", h, tt)], key=obk)
    return P.finish()


def proj_residual(P, W, kc, actT, akey, xin, xout, T, ws, pp, tsl0=0, nfb=16, wrows0=0):
    xb = Rot(P, "xb" + akey, [128, T], F32, 2)
    for cb in range(nfb // 4):
        wb, wkey = ws.load(W, cb * 512, 512, r0=wrows0)
        for m in range(4):
            f = cb * 4 + m
            x_t, xk = xb.next()
            P.dma("sp", x_t[:], xin[f * 128:(f + 1) * 128, tsl0:tsl0 + T], writes=[xk])
            for n in range(T // 512):
                ps, pk = pp.next()
                for k in range(kc):
                    mm(P, ps[:], wb[:, k, m * 128:(m + 1) * 128], actT[:, k, n * 512:(n + 1) * 512], k == 0, k == kc - 1,
                       [wkey, (akey, k)], [pk])
                P.op("dve", lambda e, x_t=x_t, ps=ps, n=n: e.tensor_tensor(out=x_t[:, n * 512:(n + 1) * 512], in0=ps[:],
                                                                           in1=x_t[:, n * 512:(n + 1) * 512], op=ALU.add),
                     reads=[pk, xk], writes=[xk])
            P.dma("sp", xout[f * 128:(f + 1) * 128, tsl0:tsl0 + T], x_t[:], reads=[xk], writes=[("xo", f, tsl0)], key=xk)


def phase_E(C):
    nc = C["nc"]
    P = Prog(nc, "E")
    merged = P.sb("merged", [128, KC, TO], BF16)
    oT = P.sb("oT", [128, 8, TO], BF16)
    ws8 = WStream(P, 8, tag="w8")
    pp = Rot(P, "pp", [128, 512], F32, 4, psum=True)
    gt = Rot(P, "gt", [128, TO], BF16, 2)
    tmp = Rot(P, "tmp", [128, 512], BF16, 3)
    for b, (OT, Wup) in enumerate(((C["OAT"], C["w_up_a"]), (C["OBT"], C["w_up_b"]), (C["OCT"], C["w_up_c"]))):
        P.dma("sp", oT[:], OT.rearrange("(kc p) t -> p kc t", p=128), writes=["oT"])
        for cb in range(4):
            wb, wkey = ws8.load(Wup, cb * 512, 512)
            for m in range(4):
                f = cb * 4 + m
                g_t, gk = gt.next()
                P.dma("sp", g_t[:], C["GT"][b * 2048 + f * 128:b * 2048 + (f + 1) * 128, :], writes=[gk])
                for n in range(4):
                    sl = slice(n * 512, (n + 1) * 512)
                    ps, pk = pp.next()
                    for k in range(8):
                        mm(P, ps[:], wb[:, k, m * 128:(m + 1) * 128], oT[:, k, sl], k == 0, k == 7, [wkey, "oT"], [pk])
                    if b == 0:
                        P.op("dve", lambda e, ps=ps, g_t=g_t, sl=sl, f=f: e.tensor_tensor(out=merged[:, f, sl], in0=ps[:], in1=g_t[:, sl], op=ALU.mult),
                             reads=[pk, gk], writes=[("merged", f)])
                    else:
                        t_t, tk = tmp.next()
                        P.op("dve", lambda e, ps=ps, g_t=g_t, sl=sl, t_t=t_t: e.tensor_tensor(out=t_t[:], in0=ps[:], in1=g_t[:, sl], op=ALU.mult),
                             reads=[pk, gk], writes=[tk])
                        P.op("pool", lambda e, t_t=t_t, sl=sl, f=f: e.tensor_tensor(out=merged[:, f, sl], in0=merged[:, f, sl], in1=t_t[:], op=ALU.add),
                             reads=[tk, ("merged", f)], writes=[("merged", f)])
    ws = WStream(P, KC, tag="w16")
    proj_residual(P, C["w_o"], KC, merged, "merged", C["xT_own"], C["X1T"], TO, ws, pp)
    return P.finish()


def phase_F1(C):
    nc = C["nc"]
    P = Prog(nc, "F1")
    ones = make_ones(P)
    gm = load_small(P, "gs_mT", C["norm_mem"], [128, KC])
    gx = load_small(P, "gs_hT", C["norm_x"], [128, KC])
    scr = rms_scratch(P, KC, 512, F32)
    mT = P.sb("mT", [128, KC, MEM], BF16)
    rms_fm(P, C["memT"], KC, MEM, gm, mT, "mT", scr, ones, tw=256)
    ws = WStream(P, KC, tag="w16")
    pp = Rot(P, "pp", [128, 512], F32, 4, psum=True)
    st = Rot(P, "st", [128, 512], BF16, 4)
    cnt = 0
    for cb in range(4):
        wb, wkey = ws.load(C["w_xkv"], cb * 512, 512)
        for m in range(4):
            f = cb * 4 + m
            ps, pk = pp.next()
            for k in range(KC):
                mm(P, ps[:, 0:MEM], wb[:, k, m * 128:(m + 1) * 128], mT[:, k, :], k == 0, k == KC - 1, [wkey, ("mT", k)], [pk])
            so, sk = st.next()
            cnt += 1
            evac_copy(P, cnt, so[:, 0:MEM], ps[:, 0:MEM], [pk], [sk])
            P.dma("sp", C["KXT"][f * 128:(f + 1) * 128, :], so[:, 0:MEM], reads=[sk], writes=[("kxt", f)], key=sk)
    for cb in range(4):
        wb, wkey = ws.load(C["w_xkv"], 2048 + cb * 512, 512)
        for mb in range(2):
            ps, pk = pp.next()
            for k in range(KC):
                mm(P, ps[:], mT[:, k, mb * 128:(mb + 1) * 128], wb[:, k, :], k == 0, k == KC - 1, [wkey, ("mT", k)], [pk])
            so, sk = st.next()
            cnt += 1
            evac_copy(P, cnt, so[:], ps[:], [pk], [sk])
            P.dma("sp", C["VX"][mb * 128:(mb + 1) * 128, cb * 512:(cb + 1) * 512], so[:], reads=[sk], writes=[("vx", mb, cb)], key=sk)
    hT = P.sb("hT", [128, KC, TO], BF16)
    rms_fm(P, C["X1T"], KC, TO, gx, hT, "hT", scr, ones)
    for cb in range(4):
        wb, wkey = ws.load(C["w_xq"], cb * 512, 512)
        for m in range(4):
            f = cb * 4 + m
            for n in range(4):
                ps, pk = pp.next()
                for k in range(KC):
                    mm(P, ps[:], wb[:, k, m * 128:(m + 1) * 128], hT[:, k, n * 512:(n + 1) * 512], k == 0, k == KC - 1, [wkey, ("hT", k)], [pk])
                so, sk = st.next()
                cnt += 1
                evac_copy(P, cnt, so[:], ps[:], [pk], [sk])
                P.dma("sp", C["QXT"][f * 128:(f + 1) * 128, n * 512:(n + 1) * 512], so[:], reads=[sk], writes=[("qxt", f, n)], key=sk)
    return P.finish()


def phase_F2(C):
    nc = C["nc"]
    P = Prog(nc, "F2")
    ones = make_ones(P)
    KxT = P.sb("KxT", [128, 16, MEM], BF16)
    Vx = P.sb("Vx", [128, 2, 2048], BF16)
    P.dma("sp", KxT[:], C["KXT"].rearrange("(f p) m -> p f m", p=128), writes=["KxT"])
    P.dma("sp", Vx[:], C["VX"].rearrange("(mb p) c -> p mb c", p=128), writes=["Vx"])
    ws = WStream(P, KC, tag="w16")
    pp = Rot(P, "pp", [128, 512], F32, 4, psum=True)
    oxT = P.sb("oxT", [128, KC, TO], BF16)
    qh = Rot(P, "qh", [128, 4, TO], BF16, 2)
    pT = Rot(P, "pT", [128, 2, 512], BF16, 2)
    rd = Rot(P, "rd", [128, 512], F32, 2)
    scale = 512 ** -0.5
    for h in range(4):
        q_t, qk = qh.next()
        P.dma("sp", q_t[:], C["QXT"][h * 512:(h + 1) * 512, :].rearrange("(dc p) t -> p dc t", p=128), writes=[qk])
        for n in range(4):
            sl = slice(n * 512, (n + 1) * 512)
            pt, ptk = pT.next()
            for mb in range(2):
                ps, pk = pp.next()
                for dc in range(4):
                    mm(P, ps[:], KxT[:, h * 4 + dc, mb * 128:(mb + 1) * 128], q_t[:, dc, sl], dc == 0, dc == 3, ["KxT", qk], [pk])
                P.op("act", lambda e, pt=pt, ps=ps, mb=mb: e.activation(out=pt[:, mb, :], in_=ps[:], func=AF.Exp, scale=scale), reads=[pk], writes=[ptk])
            ps, pk = pp.next()
            for mb in range(2):
                mm(P, ps[:], ones[:], pt[:, mb, :], mb == 0, mb == 1, ["ones", ptk], [pk])
            r_t, rk = rd.next()
            P.op("dve", lambda e, r_t=r_t, ps=ps: e.reciprocal(out=r_t[:], in_=ps[:]), reads=[pk], writes=[rk])
            for dc in range(4):
                ps, pk = pp.next()
                for mb in range(2):
                    mm(P, ps[:], Vx[:, mb, h * 512 + dc * 128:h * 512 + (dc + 1) * 128], pt[:, mb, :], mb == 0, mb == 1, ["Vx", ptk], [pk])
                P.op("dve", lambda e, ps=ps, r_t=r_t, f=h * 4 + dc, sl=sl: e.tensor_tensor(out=oxT[:, f, sl], in0=ps[:], in1=r_t[:], op=ALU.mult),
                     reads=[pk, rk], writes=[("oxT", h * 4 + dc)])
    proj_residual(P, C["w_xo"], KC, oxT, "oxT", C["X1T"], C["X2T"], TO, ws, pp)
    return P.finish()


def phase_G2(C):
    nc = C["nc"]
    P = Prog(nc, "G2")
    ones = make_ones(P)
    gx = load_small(P, "gs_hT", C["norm_ffn"], [128, KC])
    hT = P.sb("hT", [128, KC, TO], BF16)
    rms_fm(P, C["X2T"], KC, TO, gx, hT, "hT", rms_scratch(P, KC, 256, F32), ones, tw=256)
    wsg = WStream(P, KC, tag="wg")
    wsu = WStream(P, KC, tag="wu")
    pg = Rot(P, "pg", [128, 512], F32, 3, psum=True)
    pu = Rot(P, "pu", [128, 512], F32, 3, psum=True)
    sg = Rot(P, "sg", [128, 512], F32, 3)
    st = Rot(P, "st", [128, 512], BF16, 4)
    for cb in range(DFF // 512):
        wg, wgk = wsg.load(C["w_ffn_gate"], cb * 512, 512)
        wu, wuk = wsu.load(C["w_ffn_up"], cb * 512, 512)
        for m in range(4):
            f = cb * 4 + m
            for n in range(4):
                sl = slice(n * 512, (n + 1) * 512)
                p1, p1k = pg.next()
                p2, p2k = pu.next()
                for k in range(KC):
                    mm(P, p1[:], wg[:, k, m * 128:(m + 1) * 128], hT[:, k, sl], k == 0, k == KC - 1, [wgk, ("hT", k)], [p1k])
                for k in range(KC):
                    mm(P, p2[:], wu[:, k, m * 128:(m + 1) * 128], hT[:, k, sl], k == 0, k == KC - 1, [wuk, ("hT", k)], [p2k])
                s_t, sk = sg.next()
                P.op("act", lambda e, s_t=s_t, p1=p1: e.activation(out=s_t[:], in_=p1[:], func=AF.Silu), reads=[p1k], writes=[sk])
                o_t, ok = st.next()
                P.op("dve", lambda e, o_t=o_t, s_t=s_t, p2=p2: e.tensor_tensor(out=o_t[:], in0=p2[:], in1=s_t[:], op=ALU.mult), reads=[p2k, sk], writes=[ok])
                P.dma("sp", C["ACTT"][f * 128:(f + 1) * 128, sl], o_t[:], reads=[ok], writes=[("actt", f, n)], key=ok)
    return P.finish()


def phase_G3(C):
    nc = C["nc"]
    P = Prog(nc, "G3")
    NK = DFF // 128
    ws = WStream(P, NK, tag="wd")
    pp = Rot(P, "pp", [128, 512], F32, 4, psum=True)
    aT = Rot(P, "aT", [128, NK, 512], BF16, 2)
    xb = Rot(P, "xb", [128, 512], F32, 3)
    for cb in range(4):
        wb, wkey = ws.load(C["w_ffn_down"], cb * 512, 512)
        for tg in range(TO // 512):
            tsl = slice(tg * 512, (tg + 1) * 512)
            a_t, ak = aT.next()
            for k0 in range(0, NK, 11):
                P.dma("sp", a_t[:, k0:k0 + 11, :], C["ACTT"][k0 * 128:(k0 + 11) * 128, tsl].rearrange("(kc p) t -> p kc t", p=128), writes=[ak])
            for m in range(4):
                f = cb * 4 + m
                x_t, xk = xb.next()
                P.dma("sp", x_t[:], C["X2T"][f * 128:(f + 1) * 128, tsl], writes=[xk])
                ps, pk = pp.next()
                for k in range(NK):
                    mm(P, ps[:], wb[:, k, m * 128:(m + 1) * 128], a_t[:, k, :], k == 0, k == NK - 1, [wkey, ak], [pk])
                P.op("dve", lambda e, x_t=x_t, ps=ps: e.tensor_tensor(out=x_t[:], in0=ps[:], in1=x_t[:], op=ALU.add), reads=[pk, xk], writes=[xk])
                P.dma("sp", C["X3T"][f * 128:(f + 1) * 128, tsl], x_t[:], reads=[xk], writes=[("x3", f, tg)], key=xk)
                if "XC" in C:
                    P.dma("sp", C["XC"][f, tg // 2, :, (tg % 2) * 512:(tg % 2 + 1) * 512], x_t[:], reads=[xk], writes=[("xc", f, tg)], key=xk)
    return P.finish()


def phase_H(C):
    nc = C["nc"]
    P = Prog(nc, "H")
    ones = make_ones(P)
    gs = load_small(P, "gs", C["norm_final"], [128, KC])
    xs = P.sb("xs", [128, KC, 512], F32)
    sq = P.sb("sq", [128, KC, 512], BF16)
    rstd = P.sb("rstd", [128, 512], F32)
    pss = P.ps("pss", [128, 512])
    oo = Rot(P, "oo", [128, 512], F32, 4)
    for n in range(TO // 512):
        sl = slice(n * 512, (n + 1) * 512)
        for k0 in (0, 8):
            P.dma("sp", xs[:, k0:k0 + 8, :], C["X3T"][k0 * 128:(k0 + 8) * 128, sl].rearrange("(kc p) t -> p kc t", p=128),
                  writes=[("xs", k) for k in range(KC)])
        for k in range(KC):
            P.op("act", lambda e, k=k: e.activation(out=sq[:, k, :], in_=xs[:, k, :], func=AF.Square), reads=[("xs", k)], writes=[("sq", k)])
        for k in range(KC):
            mm(P, pss[:], ones[:], sq[:, k, :], k == 0, k == KC - 1, ["ones", ("sq", k)], ["pss"])
        P.op("act", lambda e: e.activation(out=rstd[:], in_=pss[:], func=AF.Sqrt, scale=1.0 / D, bias=EPS), reads=["pss"], writes=["rstd"])
        P.op("dve", lambda e: e.reciprocal(out=rstd[:], in_=rstd[:]), reads=["rstd"], writes=["rstd"])
        for k in range(KC):
            o_t, ok = oo.next()
            P.op("dve", lambda e, k=k, o_t=o_t: e.scalar_tensor_tensor(out=o_t[:], in0=xs[:, k, :], scalar=gs[:, k:k + 1], in1=rstd[:],
                                                                       op0=ALU.mult, op1=ALU.mult), reads=[("xs", k), "gs", "rstd"], writes=[ok])
            P.dma("sp", C["OUTT"][k * 128:(k + 1) * 128, sl], o_t[:], reads=[ok], writes=[("out", k, n)], key=ok)
    return P.finish()


LAYER_W = [
    ("norm_mix", [128, KC]), ("w_in", [D, DIN]), ("b_gate", [128, 48]),
    ("w_alpha", [16, 512]), ("b_alpha", [1, 512]), ("gla_norm", [128, 8]),
    ("q_a_norm", [128, 4]), ("w_qb", [512, 1536]), ("kv_a_norm", [128, 4]), ("w_kvb", [512, 2048]),
    ("w_up_a", [1024, D]), ("w_up_b", [1024, D]), ("w_up_c", [1024, D]), ("w_o", [D, D]),
    ("norm_x", [128, KC]), ("norm_mem", [128, KC]), ("w_xq", [D, D]), ("w_xkv", [D, 2 * D]), ("w_xo", [D, D]),
    ("norm_ffn", [128, KC]), ("w_ffn_gate", [D, DFF]), ("w_ffn_up", [D, DFF]), ("w_ffn_down", [DFF, D]),
]
SHARED_IN = [
    ("xT_own", [D, TO], F32), ("xT_prev", [D, TO], F32), ("memT", [D, MEM], F32), ("norm_final", [128, KC], F32),
    ("prevbias", [128, 1], F32), ("hasprev", [128, 1], F32), ("amask", [128, 2, 128], F32), ("abias", [128, 48, 128], F32),
    ("cmask", [128, 4, 512], BF16), ("cos2T", [64, TC], F32), ("sin2sT", [64, TC], F32),
    ("ubd", [128, 128], F32), ("ust", [128, 128], F32), ("rowmask", [128, 2], F32),
]
SCRATCH = [
    ("AQT", [1024, TO], BF16), ("AKT", [1024, TC], BF16), ("AV", [8, TC, 128], BF16),
    ("BQT", [512, TO], BF16), ("BKT", [512, TO], BF16), ("BK", [TC, 512], BF16), ("BV", [TC, 1024], BF16),
    ("BLRT", [16, TC], F32), ("BRT", [1024, TO], BF16), ("CQAT", [512, TO], BF16), ("CKVAT", [512, TC], BF16),
    ("CKRT", [64, TC], BF16), ("CKRST", [64, TC], BF16), ("GT", [6144, TO], BF16),
    ("OAT", [1024, TO], BF16), ("OBT", [1024, TO], BF16), ("OCT", [1024, TO], BF16),
    ("X1T", [D, TO], F32), ("X2T", [D, TO], F32), ("ACTT", [DFF, TO], BF16),
    ("KXT", [D, MEM], BF16), ("VX", [MEM, D], BF16), ("QXT", [D, TO], BF16),
    ("XMID", [D, TO], F32), ("XMIDC", [16, 2, 128, 1024], F32), ("XG", [16, 2, 256, 1024], F32), ("XLAST", [D, TO], F32),
]
PHASES = ["A0", "A1", "B", "C", "D", "E", "F1", "F2", "G2", "G3"]
PAIRS = [[0, 1], [2, 3], [4, 5], [6, 7]]


def phase_X(C0):
    nc = C0["nc"]
    P = Prog(nc, "X")
    groups = C0["groups"]
    for f in range(16):
        for ch in range(2):
            P.custom("pool", lambda e, f=f, ch=ch: e.collective_compute(
                "AllGather", ALU.bypass, replica_groups=groups, ins=[C0["XMIDC"][f, ch].opt()], outs=[C0["XG"][f, ch].opt()]),
                writes=[("XG", f, ch)], key="cc")
    return P.finish()


def build_fused(groups=PAIRS):
    nc = bass.Bass("TRN2", target_bir_lowering=False)
    _POOLS[id(nc)] = SemPool(nc)
    base = {"nc": nc, "groups": groups}
    for name, shape, dt in SHARED_IN:
        base[name] = nc.dram_tensor(name, shape, dt, kind="ExternalInput").ap()
    for name, shape, dt in SCRATCH:
        base[name] = nc.dram_tensor(name, shape, dt, kind="Internal").ap()
    base["OUTT"] = nc.dram_tensor("OUTT", [D, TO], F32, kind="ExternalOutput").ap()
    Cs = []
    for l in range(2):
        C = dict(base)
        for name, shape in LAYER_W:
            C[name] = nc.dram_tensor(f"{name}_{l}", shape, F32, kind="ExternalInput").ap()
        Cs.append(C)
    Cs[0]["X3T"] = base["XMID"]
    Cs[0]["XC"] = base["XMIDC"]
    Cs[1]["xT_own"] = base["XMID"]
    XG = base["XG"]
    Cs[1]["xT_prev"] = lambda k0, k1, t0, tw: XG[k0:k1, t0 // 1024, 0:128, (t0 % 1024):(t0 % 1024) + tw].rearrange("kc p t -> p kc t")
    Cs[1]["X3T"] = base["XLAST"]
    for l, C in enumerate(Cs):
        fns = {"A0": lambda: phase_A(C, 0), "A1": lambda: phase_A(C, 1), "B": lambda: phase_B(C), "C": lambda: phase_C(C),
               "D": lambda: phase_D(C), "E": lambda: phase_E(C), "F1": lambda: phase_F1(C), "F2": lambda: phase_F2(C),
               "G2": lambda: phase_G2(C), "G3": lambda: phase_G3(C)}
        for ph in PHASES:
            n = fns[ph]()
            if os.environ.get("K_VERBOSE"):
                print("layer", l, "phase", ph, "instr", n, "sems", _POOLS[id(nc)].n, flush=True)
        if l == 0:
            phase_X(C)
    phase_H(Cs[1])
    _POOLS[id(nc)].close()
    return nc


def _t5_bucket(dist):
    dist = np.asarray(dist)
    n = np.maximum(dist, 1).astype(np.float32)
    large = 16 + (np.log(n / np.float32(16)) / np.float32(math.log(2048 / 16)) * np.float32(16)).astype(np.int32)
    large = np.minimum(large, 31)
    return np.where(dist < 16, dist, large)


def _vec(v, n):
    return np.ascontiguousarray(np.asarray(v, np.float32).reshape(n, 128).T)


def _constants():
    k = np.arange(128)[:, None]
    q = np.arange(128)[None, :]
    amask = np.stack([(k >= q), (k <= q)], axis=1).astype(np.float32)
    steps = np.stack([q + 128 - k, q - k], axis=1)
    steps = np.clip(steps, 0, 128)
    qq = np.arange(512)[None, None, :]
    dj = np.arange(4)[None, :, None]
    import ml_dtypes
    cmask = ((k[:, :, None] + 128 * dj) <= qq).astype(np.float32).astype(ml_dtypes.bfloat16)
    same = (k // 64) == (q // 64)
    ubd = (same & (k <= q)).astype(np.float32)
    ust = (same & (k > q)).astype(np.float32)
    p = np.arange(128)
    rowmask = np.stack([(p < 64), (p >= 64)], axis=1).astype(np.float32)
    return amask, steps, cmask, ubd, ust, rowmask


def _rope_tables(half):
    pos = (np.arange(TC) + (half - 1) * TO).astype(np.float32)
    inv = (np.float32(10000.0) ** (-np.arange(0, 64, 2, dtype=np.float32) / np.float32(64))).astype(np.float32)
    ang = pos[None, :] * inv[:, None]
    cos, sin = np.cos(ang).astype(np.float32), np.sin(ang).astype(np.float32)
    cos2 = np.concatenate([cos, cos], 0)
    sin2s = np.concatenate([-sin, sin], 0)
    return np.ascontiguousarray(cos2), np.ascontiguousarray(sin2s)


_NC_CACHE = {}


def _get_nc(groups=PAIRS):
    key = tuple(tuple(g) for g in groups)
    if key not in _NC_CACHE:
        _NC_CACHE[key] = build_fused(groups)
    return _NC_CACHE[key]


def make_inputs(inp, cores):
    amask, steps, cmask, ubd, ust, rowmask = _constants()
    rel = np.asarray(inp["rel_bias"], np.float32)
    ab = np.zeros((128, 48, 128), np.float32)
    for g, dil in enumerate((1, 4, 16)):
        bk = _t5_bucket(steps * dil)
        for h in range(8):
            ab[:, (g * 8 + h) * 2:(g * 8 + h) * 2 + 2, :] = rel[bk, h]
    shared = {"norm_final": _vec(inp["norm_final"], KC), "amask": amask, "abias": ab, "cmask": cmask, "ubd": ubd, "ust": ust,
              "rowmask": rowmask}
    for l in range(2):
        lw = {
            "norm_mix": _vec(inp["norm_mix"][l], KC), "w_in": np.asarray(inp["w_in"][l]), "b_gate": _vec(inp["b_gate"][l], 48),
            "w_alpha": np.asarray(inp["w_alpha"][l]), "b_alpha": np.asarray(inp["b_alpha"][l]).reshape(1, 512),
            "gla_norm": np.ascontiguousarray(np.asarray(inp["gla_norm"][l], np.float32).reshape(4, 2, 128).transpose(2, 0, 1).reshape(128, 8)),
            "q_a_norm": _vec(inp["q_a_norm"][l], 4), "w_qb": np.asarray(inp["w_qb"][l]),
            "kv_a_norm": _vec(inp["kv_a_norm"][l], 4), "w_kvb": np.asarray(inp["w_kvb"][l]),
            "w_up_a": np.asarray(inp["w_up_a"][l]), "w_up_b": np.asarray(inp["w_up_b"][l]), "w_up_c": np.asarray(inp["w_up_c"][l]),
            "w_o": np.asarray(inp["w_o"][l]), "norm_x": _vec(inp["norm_x"][l], KC), "norm_mem": _vec(inp["norm_mem"][l], KC),
            "w_xq": np.asarray(inp["w_xq"][l]), "w_xkv": np.asarray(inp["w_xkv"][l]), "w_xo": np.asarray(inp["w_xo"][l]),
            "norm_ffn": _vec(inp["norm_ffn"][l], KC), "w_ffn_gate": np.asarray(inp["w_ffn_gate"][l]),
            "w_ffn_up": np.asarray(inp["w_ffn_up"][l]), "w_ffn_down": np.asarray(inp["w_ffn_down"][l]),
        }
        for k, v in lw.items():
            shared[f"{k}_{l}"] = v
    x = np.asarray(inp["x"], np.float32)
    xT = {c: np.ascontiguousarray(x[c // 2, (c % 2) * TO:(c % 2 + 1) * TO, :].T) for c in range(8)}
    zeros = np.zeros((D, TO), np.float32)
    maps = []
    for c in cores:
        b, hf = c // 2, c % 2
        cos2, sin2s = _rope_tables(hf)
        m = dict(shared)
        m["xT_own"] = xT[c]
        m["xT_prev"] = xT[c - 1] if hf == 1 else zeros
        m["memT"] = np.ascontiguousarray(np.asarray(inp["mem"][b], np.float32).T)
        m["prevbias"] = np.full((128, 1), 0.0 if hf == 1 else NEGB, np.float32)
        m["hasprev"] = np.full((128, 1), 1.0 if hf == 1 else 0.0, np.float32)
        m["cos2T"] = cos2
        m["sin2sT"] = sin2s
        maps.append(m)
    return maps


def kernel(**inp):
    cores = list(range(8))
    nc = _get_nc()
    maps = make_inputs(inp, cores)
    res = run_bass_kernel_spmd(nc, maps, core_ids=cores)
    out = np.empty((4, 4096, D), np.float32)
    for c in cores:
        out[c // 2, (c % 2) * TO:(c % 2 + 1) * TO, :] = np.asarray(res.results[c]["OUTT"]).T
    return out
```
